# Optimizing a Trainium2 kernel written in Bass

```python
import jax, jax.numpy as jnp
from jax import lax
import numpy as np

D_MODEL = 2048
BATCH = 8
SEQ = 2048
DEPTH = 2

HEAD_DIM = 128
A_HEADS_PER_GROUP = 4
DILATED_GROUPS = ((128, 1), (512, 4), (2048, 16))
A_HEADS = A_HEADS_PER_GROUP * len(DILATED_GROUPS)
A_WIDTH = A_HEADS * HEAD_DIM
A_OUT = A_HEADS_PER_GROUP * HEAD_DIM
ATT_BLOCK = 128
ROPE_THETA = 10000.0
GLA_HEADS = 4
GLA_DK = 128
GLA_DV = 256
GLA_KEY = GLA_HEADS * GLA_DK
GLA_VAL = GLA_HEADS * GLA_DV
GLA_LOWRANK = 16
GLA_TAU = 16.0
GLA_CHUNK = 64
IN_WIDTHS = (A_WIDTH, A_WIDTH, A_WIDTH, GLA_KEY, GLA_KEY, GLA_VAL, GLA_VAL, GLA_LOWRANK, D_MODEL, D_MODEL)
IN_WIDTH = sum(IN_WIDTHS)
N_EXPERTS = 16
N_GROUPS = 4
EXPERTS_PER_GROUP = N_EXPERTS // N_GROUPS
TOPK_GROUPS = 1
TOP_K = 2
D_FF_EXPERT = 1024
EXPERT_BLOCK = 128
NORM_EPS = 1e-6

kernel_name = 'hybrid_dilated_gla_grouped_moe_adaln'


def rms_norm(x, gain):
    x32 = x.astype(jnp.float32)
    y = x32 * lax.rsqrt(jnp.mean(x32 * x32, axis=-1, keepdims=True) + NORM_EPS)
    return (y * gain.astype(jnp.float32)).astype(x.dtype)


def rope_tables(seq):
    pos = jnp.arange(seq, dtype=jnp.float32)
    inv_freq = ROPE_THETA ** (-jnp.arange(0, HEAD_DIM, 2, dtype=jnp.float32) / HEAD_DIM)
    ang = pos[:, None] * inv_freq[None, :]
    return jnp.cos(ang), jnp.sin(ang)


def apply_rope(t, cos, sin):
    t1, t2 = jnp.split(t, 2, axis=-1)
    c = cos[None, :, None, :]
    s = sin[None, :, None, :]
    return jnp.concatenate([t1 * c - t2 * s, t2 * c + t1 * s], axis=-1)


def dilated_window_attention(q, k, v, window, dilation):
    B, S, H, Dh = q.shape
    L = S // dilation
    nb = -(-L // ATT_BLOCK)
    Lp = nb * ATT_BLOCK

    def to_sub(t):
        return t.reshape(B, L, dilation, H, Dh).transpose(0, 2, 3, 1, 4)

    qs = jnp.pad(to_sub(q), ((0, 0), (0, 0), (0, 0), (0, Lp - L), (0, 0)))
    qs = qs.reshape(B, dilation, H, nb, ATT_BLOCK, Dh)
    kp = jnp.pad(to_sub(k), ((0, 0), (0, 0), (0, 0), (ATT_BLOCK, Lp - L), (0, 0)))
    vp = jnp.pad(to_sub(v), ((0, 0), (0, 0), (0, 0), (ATT_BLOCK, Lp - L), (0, 0)))

    def band(t):
        prev = t[:, :, :, :Lp].reshape(B, dilation, H, nb, ATT_BLOCK, Dh)
        cur = t[:, :, :, ATT_BLOCK:].reshape(B, dilation, H, nb, ATT_BLOCK, Dh)
        return jnp.concatenate([prev, cur], axis=-2)

    kb, vb = band(kp), band(vp)
    s = jnp.einsum('brhnid,brhnjd->brhnij', qs, kb) * (Dh ** -0.5)
    i = jnp.arange(ATT_BLOCK)[:, None]
    j = jnp.arange(2 * ATT_BLOCK)[None, :]
    dist = ATT_BLOCK + i - j
    kpos = (jnp.arange(nb)[:, None, None] - 1) * ATT_BLOCK + j[None]
    mask = (dist >= 0) & (dist <= window // dilation) & (kpos >= 0)
    s = jnp.where(mask, s, -jnp.inf)
    m = jnp.max(s, axis=-1, keepdims=True)
    p = jnp.exp(s - m)
    den = jnp.sum(p, axis=-1)
    o = jnp.einsum('brhnij,brhnjd->brhnid', p, vb) / den[..., None]
    lse = m[..., 0] + jnp.log(den)
    o = o.reshape(B, dilation, H, Lp, Dh)[:, :, :, :L].transpose(0, 3, 1, 2, 4).reshape(B, S, H, Dh)
    lse = lse.reshape(B, dilation, H, Lp)[:, :, :, :L].transpose(0, 3, 1, 2).reshape(B, S, H)
    return o, lse


def gated_linear_attention(q, k, v, log_a):
    B, S, H, Dk = q.shape
    Dv = v.shape[-1]
    N = S // GLA_CHUNK

    def to_chunks(t):
        return t.reshape(B, N, GLA_CHUNK, H, t.shape[-1]).transpose(0, 3, 1, 2, 4)

    q, k, v, log_a = to_chunks(q), to_chunks(k), to_chunks(v), to_chunks(log_a)
    q = q * (Dk ** -0.5)
    b = jnp.cumsum(log_a, axis=-2)
    q_dec = q * jnp.exp(b)
    k_inv = k * jnp.exp(-b)
    causal = jnp.tril(jnp.ones((GLA_CHUNK, GLA_CHUNK), dtype=bool))
    attn = jnp.where(causal, jnp.einsum('bhnid,bhnjd->bhnij', q_dec, k_inv), 0.0)
    o_intra = jnp.einsum('bhnij,bhnjv->bhniv', attn, v)
    b_end = b[..., -1, :]
    k_end = k * jnp.exp(b_end[..., None, :] - b)
    d_state = jnp.einsum('bhncd,bhncv->bhndv', k_end, v)

    def step(state, inp):
        decay, ds = inp
        return decay[..., None] * state + ds, state

    _, s_prev = lax.scan(step, jnp.zeros((B, H, Dk, Dv), jnp.float32),
                         (jnp.moveaxis(jnp.exp(b_end), 2, 0), jnp.moveaxis(d_state, 2, 0)))
    s_prev = jnp.moveaxis(s_prev, 0, 2)
    o = o_intra + jnp.einsum('bhnid,bhndv->bhniv', q_dec, s_prev)
    return o.transpose(0, 2, 3, 1, 4).reshape(B, S, H, Dv)


def token_mixer(h, w_in, w_alpha2, b_alpha2, gla_gain, w_out_a, w_out_b, w_out, cos, sin):
    B, S, _ = h.shape
    f32 = jnp.float32
    proj = h @ w_in
    splits = np.cumsum(IN_WIDTHS)[:-1].tolist()
    q_a, k_a, v_a, q_b, k_b, v_b, r_b, a_low, g_a, g_b = jnp.split(proj, splits, axis=-1)

    q_a = apply_rope(q_a.astype(f32).reshape(B, S, A_HEADS, HEAD_DIM), cos, sin)
    k_a = apply_rope(k_a.astype(f32).reshape(B, S, A_HEADS, HEAD_DIM), cos, sin)
    v_a = v_a.astype(f32).reshape(B, S, A_HEADS, HEAD_DIM)
    outs, lses = [], []
    for gi, (window, dilation) in enumerate(DILATED_GROUPS):
        hs = slice(gi * A_HEADS_PER_GROUP, (gi + 1) * A_HEADS_PER_GROUP)
        o, lse = dilated_window_attention(q_a[:, :, hs], k_a[:, :, hs], v_a[:, :, hs], window, dilation)
        outs.append(o)
        lses.append(lse)
    wts = jax.nn.softmax(jnp.stack(lses), axis=0)
    y_a = jnp.einsum('gbsh,gbshd->bshd', wts, jnp.stack(outs)).reshape(B, S, A_OUT).astype(h.dtype)

    log_a = jax.nn.log_sigmoid((a_low @ w_alpha2 + b_alpha2).astype(f32)) / GLA_TAU
    o_b = gated_linear_attention(q_b.astype(f32).reshape(B, S, GLA_HEADS, GLA_DK),
                                 k_b.astype(f32).reshape(B, S, GLA_HEADS, GLA_DK),
                                 v_b.astype(f32).reshape(B, S, GLA_HEADS, GLA_DV),
                                 log_a.reshape(B, S, GLA_HEADS, GLA_DK))
    o_b = o_b * lax.rsqrt(jnp.mean(o_b * o_b, axis=-1, keepdims=True) + NORM_EPS)
    o_b = o_b * gla_gain.astype(f32).reshape(GLA_HEADS, GLA_DV)
    y_b = (o_b.reshape(B, S, GLA_VAL) * jax.nn.silu(r_b.astype(f32))).astype(h.dtype)

    merged = jax.nn.sigmoid(g_a) * (y_a @ w_out_a) + jax.nn.sigmoid(g_b) * (y_b @ w_out_b)
    return merged @ w_out


def route(h, w_router, b_router):
    T = h.shape[0]
    aff = jax.nn.sigmoid(h.astype(jnp.float32) @ w_router.astype(jnp.float32))
    sel = aff + b_router.astype(jnp.float32)
    grp_score = lax.top_k(sel.reshape(T, N_GROUPS, EXPERTS_PER_GROUP), 2)[0].sum(-1)
    _, top_grp = lax.top_k(grp_score, TOPK_GROUPS)
    grp_mask = jnp.sum(jax.nn.one_hot(top_grp, N_GROUPS, dtype=jnp.float32), axis=1) > 0
    expert_mask = jnp.repeat(grp_mask, EXPERTS_PER_GROUP, axis=1)
    _, idx = lax.top_k(jnp.where(expert_mask, sel, -jnp.inf), TOP_K)
    w = jnp.take_along_axis(aff, idx, axis=1)
    return w / jnp.sum(w, axis=-1, keepdims=True), idx


def moe_ffn(h, w_router, b_router, w_gate_e, w_up_e, w_down_e):
    T, D = h.shape
    gate, eidx = route(h, w_router, b_router)
    TK = T * TOP_K
    flat_e = eidx.reshape(-1)
    flat_g = gate.reshape(-1)
    order = jnp.argsort(flat_e)
    se = flat_e[order]
    tok = order // TOP_K
    counts = jnp.bincount(flat_e, length=N_EXPERTS)
    pcounts = (counts + EXPERT_BLOCK - 1) // EXPERT_BLOCK * EXPERT_BLOCK
    pend = jnp.cumsum(pcounts)
    pstart = pend - pcounts
    start = jnp.cumsum(counts) - counts
    dest = pstart[se] + jnp.arange(TK) - start[se]
    n_blk = TK // EXPERT_BLOCK + N_EXPERTS
    buf = jnp.zeros((n_blk * EXPERT_BLOCK, D), h.dtype).at[dest].set(h[tok])
    blk_e = jnp.minimum(jnp.searchsorted(pend, jnp.arange(n_blk) * EXPERT_BLOCK, side='right'),
                        N_EXPERTS - 1)

    def expert_block(args):
        xb, e = args
        return (jax.nn.silu(xb @ w_gate_e[e]) * (xb @ w_up_e[e])) @ w_down_e[e]

    ybuf = lax.map(expert_block, (buf.reshape(n_blk, EXPERT_BLOCK, D), blk_e)).reshape(-1, D)
    y = ybuf[dest] * flat_g[order][:, None].astype(h.dtype)
    return jnp.zeros_like(h).at[tok].add(y)


def setup_inputs(seed: int = 0) -> dict:
    key = jax.random.key(seed)
    ks = jax.random.split(key, 20)
    f32 = jnp.float32
    D = D_MODEL

    def nrm(k, shape, scale):
        return jax.random.normal(k, shape, f32) * scale

    return {
        'x': nrm(ks[0], (BATCH, SEQ, D), 1.0),
        'c': nrm(ks[1], (BATCH, D), 1.0),
        'w_ada': nrm(ks[2], (DEPTH, D, 6 * D), 0.5 * D ** -0.5),
        'b_ada': nrm(ks[3], (DEPTH, 6 * D), 0.02),
        'norm_mix': 1.0 + nrm(ks[4], (DEPTH, D), 0.02),
        'norm_ffn': 1.0 + nrm(ks[5], (DEPTH, D), 0.02),
        'w_in': nrm(ks[6], (DEPTH, D, IN_WIDTH), D ** -0.5),
        'w_alpha2': nrm(ks[7], (DEPTH, GLA_LOWRANK, GLA_KEY), GLA_LOWRANK ** -0.5),
        'b_alpha2': nrm(ks[8], (DEPTH, GLA_KEY), 0.1),
        'gla_gain': 1.0 + nrm(ks[9], (DEPTH, GLA_VAL), 0.02),
        'w_out_a': nrm(ks[10], (DEPTH, A_OUT, D), A_OUT ** -0.5),
        'w_out_b': nrm(ks[11], (DEPTH, GLA_VAL, D), GLA_VAL ** -0.5),
        'w_out': nrm(ks[12], (DEPTH, D, D), D ** -0.5),
        'w_router': nrm(ks[13], (D, N_EXPERTS), D ** -0.5),
        'b_router': nrm(ks[14], (N_EXPERTS,), 0.01),
        'w_gate_e': nrm(ks[15], (DEPTH, N_EXPERTS, D, D_FF_EXPERT), D ** -0.5),
        'w_up_e': nrm(ks[16], (DEPTH, N_EXPERTS, D, D_FF_EXPERT), D ** -0.5),
        'w_down_e': nrm(ks[17], (DEPTH, N_EXPERTS, D_FF_EXPERT, D), D_FF_EXPERT ** -0.5),
        'final_norm': 1.0 + nrm(ks[18], (D,), 0.02),
    }


def reference(x, c, w_ada, b_ada, norm_mix, norm_ffn, w_in, w_alpha2, b_alpha2, gla_gain,
              w_out_a, w_out_b, w_out, w_router, b_router, w_gate_e, w_up_e, w_down_e, final_norm):
    B, S, D = x.shape
    cos, sin = rope_tables(S)
    c_act = jax.nn.silu(c)
    for layer in range(DEPTH):
        ada = c_act @ w_ada[layer] + b_ada[layer]
        shift1, scale1, gate1, shift2, scale2, gate2 = jnp.split(ada[:, None, :], 6, axis=-1)
        h = rms_norm(x, norm_mix[layer]) * (1 + scale1) + shift1
        x = x + gate1 * token_mixer(h, w_in[layer], w_alpha2[layer], b_alpha2[layer], gla_gain[layer],
                                    w_out_a[layer], w_out_b[layer], w_out[layer], cos, sin)
        h = rms_norm(x, norm_ffn[layer]) * (1 + scale2) + shift2
        y = moe_ffn(h.reshape(B * S, D), w_router, b_router,
                    w_gate_e[layer], w_up_e[layer], w_down_e[layer]).reshape(B, S, D)
        x = x + gate2 * y
    return rms_norm(x, final_norm)
```

```python
import contextlib
import numpy as np
import concourse.bass as bass
import concourse.mybir as mybir
from concourse.bass_utils import run_bass_kernel_spmd

F32 = mybir.dt.float32
BF16 = mybir.dt.bfloat16
AF = mybir.ActivationFunctionType
ALU = mybir.AluOpType
AX = mybir.AxisListType

D = 2048
S_LEN = 2048
NT = 16
NC_ = 16
DEPTH = 2
IN_W = 11792
EPS = 1e-6
NEG = -1.0e9
O_QA, O_KA, O_VA, O_QB, O_KB, O_VB, O_RB, O_AL, O_GA, O_GB = 0, 1536, 3072, 4608, 5120, 5632, 6656, 7680, 7696, 9744
GROUPS = ((128, 1), (512, 4), (2048, 16))


class Op:
    __slots__ = ("eng", "fn", "waits", "signal", "dma_sem", "dma_val", "idx")

    def __init__(self, eng, fn):
        self.eng = eng
        self.fn = fn
        self.waits = []
        self.signal = False
        self.dma_sem = None
        self.dma_val = 0
        self.idx = 0


class Sched:
    def __init__(self, nc, n_dma_sems=48, same_engine_sync=True, n_sw=12):
        self.nc = nc
        self.ops = {e: [] for e in ("tensor", "vector", "scalar", "gpsimd", "sync")}
        self.last_writer = {}
        self.readers = {}
        self.n_dma_sems = n_dma_sems
        self.dma_count = [0] * n_dma_sems
        self.dma_last_tok = [None] * n_dma_sems
        self.dma_rr = 0
        self.n_sw = n_sw
        self.sw_rr = 0
        self.same_engine_sync = same_engine_sync
        self.barrier_toks = []
        self.last_compute = {}

    def _deps(self, reads, writes):
        toks = []
        for r in reads:
            w = self.last_writer.get(r)
            if w is not None:
                toks.append(w)
            if isinstance(r, tuple) and r[0] == "ps":
                toks.extend(self.readers.get(r, ()))
        for w_ in writes:
            w = self.last_writer.get(w_)
            if w is not None:
                toks.append(w)
            toks.extend(self.readers.get(w_, ()))
        return toks

    def _register(self, tok, reads, writes):
        for r in reads:
            self.readers.setdefault(r, []).append(tok)
        for w in writes:
            self.last_writer[w] = tok
            self.readers[w] = []

    def op(self, eng, fn, reads=(), writes=()):
        o = Op(eng, fn)
        o.idx = len(self.ops[eng])
        tok = ("c", eng, o.idx)
        for t in self._deps(reads, writes) + self.barrier_toks:
            if t[0] == "c" and t[1] == eng:
                if eng == "tensor" or not self.same_engine_sync:
                    continue
            o.waits.append(t)
        self.ops[eng].append(o)
        self.last_compute[eng] = tok
        self._register(tok, reads, writes)
        return tok

    def dma(self, queue, fn, reads=(), writes=()):
        o = Op(queue, fn)
        o.idx = len(self.ops[queue])
        if queue == "gpsimd":
            s = self.sw_rr
            self.sw_rr = (self.sw_rr + 1) % self.n_sw
        else:
            s = self.n_sw + self.dma_rr
            self.dma_rr = (self.dma_rr + 1) % (self.n_dma_sems - self.n_sw)
        prev = self.dma_last_tok[s]
        if prev is not None:
            o.waits.append(prev)
        self.dma_count[s] += 1
        o.dma_sem = s
        o.dma_val = 16 * self.dma_count[s]
        tok = ("d", s, o.dma_val)
        self.dma_last_tok[s] = tok
        o.waits.extend(self._deps(reads, writes))
        o.waits.extend(self.barrier_toks)
        self.ops[queue].append(o)
        self._register(tok, reads, writes)
        return tok

    def barrier(self):
        toks = [t for t in self.dma_last_tok if t is not None]
        toks.extend(self.last_compute.values())
        self.barrier_toks = toks
        self.last_writer = {}
        self.readers = {}

    def finish(self, eng="sync"):
        o = Op(eng, None)
        o.idx = len(self.ops[eng])
        o.waits = [t for t in self.dma_last_tok if t is not None]
        self.ops[eng].append(o)

    def emit(self):
        nc = self.nc
        for e, ops in self.ops.items():
            for o in ops:
                for t in o.waits:
                    if t[0] == "c":
                        self.ops[t[1]][t[2]].signal = True
        rank = {}
        for e, ops in self.ops.items():
            r = 0
            for o in ops:
                if o.signal and o.dma_sem is None:
                    r += 1
                    rank[(e, o.idx)] = r
        with contextlib.ExitStack() as st:
            esem = {e: st.enter_context(nc.semaphore("s_" + e)) for e in self.ops}
            dsem = [st.enter_context(nc.semaphore("d%d" % i)) for i in range(self.n_dma_sems)]
            block = st.enter_context(nc.Block())

            def body_for(e):
                ops = self.ops[e]

                def body(eng):
                    seen = {}
                    for o in ops:
                        need = {}
                        for t in o.waits:
                            if t[0] == "c":
                                key = ("c", t[1])
                                val = rank[(t[1], t[2])]
                            else:
                                key = ("d", t[1])
                                val = t[2]
                            if seen.get(key, 0) >= val:
                                continue
                            if need.get(key, 0) < val:
                                need[key] = val
                        for key, val in need.items():
                            sem = esem[key[1]] if key[0] == "c" else dsem[key[1]]
                            eng.wait_ge(sem, val)
                            seen[key] = val
                        if o.fn is None:
                            continue
                        inst = o.fn(eng)
                        if o.dma_sem is not None:
                            inst.then_inc(dsem[o.dma_sem], 16)
                        elif o.signal:
                            inst.then_inc(esem[e], 1)
                return body

            for e in self.ops:
                if self.ops[e]:
                    getattr(block, e)(body_for(e))


class B:
    def __init__(self, debug=(), upto=99):
        self.nc = bass.Bass("TRN2", target_bir_lowering=False)
        self.S = Sched(self.nc)
        self.debug = set(debug)
        self.upto = upto
        self.sb_off = 16384
        self.uid = 0
        self.rr = 0

    def dram_in(self, name, shape, dt=F32):
        return self.nc.dram_tensor(name, list(shape), dt, kind="ExternalInput").ap()

    def dram_out(self, name, shape, dt=F32):
        return self.nc.dram_tensor(name, list(shape), dt, kind="ExternalOutput").ap()

    def scratch(self, name, shape, dt=F32):
        kind = "ExternalOutput" if name in self.debug else "Internal"
        return self.nc.dram_tensor(name, list(shape), dt, kind=kind).ap()

    def sb(self, name, shape, dt):
        n = 1
        for s in shape[1:]:
            n *= s
        nbytes = n * (4 if dt == F32 else 2)
        off = (self.sb_off + 63) // 64 * 64
        self.uid += 1
        t = self.nc.alloc_sbuf_tensor_at("%s_%d" % (name, self.uid), list(shape), dt, offset=off)
        self.sb_off = off + nbytes
        assert self.sb_off <= 16384 + 212000, (name, self.sb_off)
        return t.ap()

    def mm(self, out, lhsT, rhs, start, stop, r, w):
        self.S.op("tensor", lambda e: e.matmul(out, lhsT=lhsT, rhs=rhs, start=start, stop=stop), r, w)

    def tr(self, out, in_, ident, r, w):
        self.S.op("tensor", lambda e: e.transpose(out=out, in_=in_, identity=ident), r, w)

    def act(self, out, in_, func, r, w, bias=None, scale=1.0, accum=None):
        def f(e):
            kw = {}
            if bias is not None:
                kw["bias"] = bias
            if accum is not None:
                kw["accum_out"] = accum
            return e.activation(out=out, in_=in_, func=func, scale=scale, **kw)
        self.S.op("scalar", f, r, w)

    def tt(self, eng, out, in0, in1, op, r, w):
        self.S.op(eng, lambda e: e.tensor_tensor(out=out, in0=in0, in1=in1, op=op), r, w)

    def ts(self, eng, out, in0, s1, s2, op0, op1, r, w):
        if op1 is None:
            self.S.op(eng, lambda e: e.tensor_scalar(out=out, in0=in0, scalar1=s1, scalar2=None, op0=op0), r, w)
        else:
            self.S.op(eng, lambda e: e.tensor_scalar(out=out, in0=in0, scalar1=s1, scalar2=s2, op0=op0, op1=op1), r, w)

    def stt(self, out, in0, scalar, in1, op0, op1, r, w, accum=None):
        def f(e):
            if accum is not None:
                return e.scalar_tensor_tensor(out=out, in0=in0, scalar=scalar, in1=in1, op0=op0, op1=op1, accum_out=accum)
            return e.scalar_tensor_tensor(out=out, in0=in0, scalar=scalar, in1=in1, op0=op0, op1=op1)
        self.S.op("vector", f, r, w)

    def cp(self, eng, out, in_, r, w):
        if eng == "scalar":
            self.act(out, in_, AF.Copy, r, w)
        else:
            self.S.op(eng, lambda e: e.tensor_copy(out=out, in_=in_), r, w)

    def cp_rr(self, out, in_, r, w):
        self.rr += 1
        self.cp("scalar" if self.rr % 2 else "vector", out, in_, r, w)

    def dma(self, q, out, in_, r, w, slow=False):
        if slow:
            self.S.dma(q, lambda e: e.dma_start(out=out, in_=in_, allow_slow_non_contiguous=True), r, w)
        else:
            self.S.dma(q, lambda e: e.dma_start(out=out, in_=in_), r, w)

    def red(self, out, in_, op, r, w):
        self.S.op("vector", lambda e: e.tensor_reduce(out=out, in_=in_, axis=AX.X, op=op), r, w)

    def recip(self, out, in_, r, w):
        self.S.op("vector", lambda e: e.reciprocal(out=out, in_=in_), r, w)

    def memset(self, eng, ap, v, r, w):
        self.S.op(eng, lambda e: e.memset(ap, v), r, w)


def host_consts():
    pos = np.arange(S_LEN, dtype=np.float32)
    inv = (np.float32(10000.0) ** (-np.arange(0, 128, 2, dtype=np.float32) / np.float32(128))).astype(np.float32)
    ang = (pos[None, :] * inv[:, None]).astype(np.float32)
    cosT = np.concatenate([np.cos(ang), np.cos(ang)], 0).astype(np.float32)
    sinT = np.concatenate([-np.sin(ang), np.sin(ang)], 0).astype(np.float32)
    import ml_dtypes
    pswap = np.zeros((128, 128), np.float32)
    for m in range(128):
        pswap[(m + 64) % 128, m] = 1.0
    ident = np.eye(128, dtype=np.float32)
    j = np.arange(128)[:, None]
    i = np.arange(128)[None, :]
    U = (j <= i).astype(np.float32)
    L = (j > i).astype(np.float32)
    ii = np.arange(128)[:, None]
    jj = np.arange(128)[None, :]
    mprev = np.where(jj >= ii, 0.0, NEG).astype(np.float32)
    mcur = np.where(jj <= ii, 0.0, NEG).astype(np.float32)
    maskA = np.concatenate([mprev, mcur], 1).astype(np.float32)
    bf = ml_dtypes.bfloat16
    return {
        "k_cosT": cosT, "k_sinT": sinT, "k_pswap": pswap.astype(bf), "k_identb": ident.astype(bf),
        "k_identf": ident, "k_Uf": U, "k_Ub": U.astype(bf), "k_Lf": L, "k_maskA": maskA,
    }


def build(debug=(), upto=99):
    b = B(debug, upto)
    nc, S = b.nc, b.S
    x_in = b.dram_in("x", [S_LEN, D])
    c_in = b.dram_in("c", [D])
    w_ada = b.dram_in("w_ada", [DEPTH, D, 6 * D])
    b_ada = b.dram_in("b_ada", [DEPTH, 6 * D])
    norm_mix = b.dram_in("norm_mix", [DEPTH, D])
    norm_ffn = b.dram_in("norm_ffn", [DEPTH, D])
    w_in = b.dram_in("w_in", [DEPTH, D, IN_W])
    w_alpha2 = b.dram_in("w_alpha2", [DEPTH, 16, 512])
    b_alpha2 = b.dram_in("b_alpha2", [DEPTH, 512])
    gla_gain = b.dram_in("gla_gain", [DEPTH, 1024])
    w_out_a = b.dram_in("w_out_a", [DEPTH, 512, D])
    w_out_b = b.dram_in("w_out_b", [DEPTH, 1024, D])
    w_out = b.dram_in("w_out", [DEPTH, D, D])
    w_router = b.dram_in("w_router", [D, 16])
    b_router = b.dram_in("b_router", [16])
    if upto >= 7:
        w_gate_e = b.dram_in("w_gate_e", [DEPTH, 16, D, 1024])
        w_up_e = b.dram_in("w_up_e", [DEPTH, 16, D, 1024])
        w_down_e = b.dram_in("w_down_e", [DEPTH, 16, 1024, D])
    final_norm = b.dram_in("final_norm", [D])
    k_cosT = b.dram_in("k_cosT", [128, S_LEN])
    k_sinT = b.dram_in("k_sinT", [128, S_LEN])
    k_pswap = b.dram_in("k_pswap", [128, 128], BF16)
    k_identb = b.dram_in("k_identb", [128, 128], BF16)
    k_identf = b.dram_in("k_identf", [128, 128])
    k_Uf = b.dram_in("k_Uf", [128, 128])
    k_Ub = b.dram_in("k_Ub", [128, 128], BF16)
    k_Lf = b.dram_in("k_Lf", [128, 128])
    k_maskA = b.dram_in("k_maskA", [128, 256])
    out = b.dram_out("out", [S_LEN, D])

    ada = b.scratch("ada", [DEPTH, 6 * D])
    xs = [x_in] + [b.scratch("xs%d" % i, [S_LEN, D]) for i in range(1, 2 * DEPTH + 1)]
    hTs = b.scratch("hTs", [128, NC_, S_LEN], BF16)
    qaT = b.scratch("qaT", [12, 128, S_LEN], BF16)
    kaT = b.scratch("kaT", [12, 128, S_LEN], BF16)
    va = b.scratch("va", [S_LEN, 1536], BF16)
    qbT = b.scratch("qbT", [4, 128, S_LEN])
    kbT = b.scratch("kbT", [4, 128, S_LEN])
    kbt = b.scratch("kbt", [S_LEN, 512])
    vb = b.scratch("vb", [S_LEN, 1024], BF16)
    rb = b.scratch("rb", [S_LEN, 1024])
    alT = b.scratch("alT", [16, S_LEN])
    sga = b.scratch("sga", [NC_, 128, S_LEN], BF16)
    sgb = b.scratch("sgb", [NC_, 128, S_LEN], BF16)
    oa = [b.scratch("oa%d" % g, [S_LEN, 4, 130]) for g in range(3)]
    yaT_d = b.scratch("yaT", [128, 4, S_LEN], BF16)
    ybT_d = b.scratch("ybT", [128, 8, S_LEN], BF16)
    gates_d = b.scratch("gates", [128, NT, 16])

    identb = b.sb("identb", [128, 128], BF16)
    identf = b.sb("identf", [128, 128], F32)
    persist_end = None
    b.dma("sync", identb, k_identb, [], ["identb"])
    b.dma("sync", identf, k_identf, [], ["identf"])
    gates_p = b.sb("gates", [128, NT, 16], F32)
    persist_end = b.sb_off

    st = contextlib.ExitStack()
    pst = [st.enter_context(nc.psum_tensor("ps%d" % i, [128, 512], F32)) for i in range(8)]
    ps = [t.ap() if hasattr(t, "ap") else t[:] for t in pst]
    psb = [p.bitcast(BF16) for p in ps]

    def PS(i):
        return ("ps", i)

    c_act = b.sb("c_act", [128, NC_], BF16)
    persist_end = b.sb_off

    def make_ada_emitter(l, banks):
        wblk = [b.sb("wada%d_%d" % (l, i), [128, NC_, 512], BF16) for i in range(2)]
        brow = [b.sb("brow%d_%d" % (l, i), [1, 512], F32) for i in range(2)]
        orow = [b.sb("orow%d_%d" % (l, i), [1, 512], F32) for i in range(2)]
        wv = w_ada[l].rearrange("(c p) n -> p c n", p=128)

        def emit(j):
            i = j % 2
            wb = wblk[i]
            wk = ("wada", l, i)
            b.dma("gpsimd", wb, wv[:, :, j * 512:(j + 1) * 512], [], [wk])
            b.dma("sync", brow[i], b_ada[l:l + 1, j * 512:(j + 1) * 512], [], [("brow", l, i)])
            p = ps[banks[i]]
            for k in range(NC_):
                b.mm(p[0:1, :], c_act[:, k:k + 1], wb[:, k, :], k == 0, k == NC_ - 1, [wk, "c_act"], [PS(banks[i])])
            b.tt("vector", orow[i], p[0:1, :], brow[i], ALU.add, [PS(banks[i]), ("brow", l, i)], [("orow", l, i)])
            b.dma("sync", ada[l:l + 1, j * 512:(j + 1) * 512], orow[i], [("orow", l, i)], [])
        return emit

    def phase_ada():
        b.sb_off = persist_end
        c_col = b.sb("c_col", [128, NC_], F32)
        b.dma("sync", c_col, c_in.rearrange("(c p) -> p c", p=128), [], ["c_col"], slow=True)
        b.act(c_act, c_col, AF.Silu, ["c_col"], ["c_act"])
        em = make_ada_emitter(0, (0, 1))
        for j in range(24):
            em(j)
        S.barrier()

    def phase_norm(l, x_src, gain_vec, col_shift, col_scale, hT, router=None, hts=None):
        G = b.sb("G_bc", [128, D], F32)
        A = b.sb("A_bc", [128, D], F32)
        Bc = b.sb("B_bc", [128, D], F32)
        NBX = 4
        xt = [b.sb("xt%d" % i, [128, D], F32) for i in range(NBX)]
        hb = [b.sb("hb%d" % i, [128, D], BF16) for i in range(2)]
        junk = b.sb("junk", [128, D], BF16)
        st_ = b.sb("nstat", [128, NT, 4], F32)
        b.dma("sync", G, gain_vec.partition_broadcast(128), [], ["G"])
        b.dma("sync", A, ada[l, col_scale * D:(col_scale + 1) * D].partition_broadcast(128), [], ["A"])
        b.dma("sync", Bc, ada[l, col_shift * D:(col_shift + 1) * D].partition_broadcast(128), [], ["Bc"])
        b.stt(A, A, 1.0, G, ALU.add, ALU.mult, ["A", "G"], ["A"])
        if router is not None:
            wr, aff_all, hTf = router

        def n1(t):
            i = t % NBX
            xk = ("xt", i)
            sk = ("nst", t)
            b.dma("sync", xt[i], x_src[t * 128:(t + 1) * 128, :], [], [xk])
            b.act(junk, xt[i], AF.Square, [xk], ["junk", sk], accum=st_[:, t, 0:1])
            b.ts("vector", st_[:, t, 1:2], st_[:, t, 0:1], 1.0 / D, EPS, ALU.mult, ALU.add, [sk], [sk])
            b.act(st_[:, t, 2:3], st_[:, t, 1:2], AF.Sqrt, [sk], [sk])
            b.recip(st_[:, t, 3:4], st_[:, t, 2:3], [sk], [sk])

        def n2(t):
            i = t % NBX
            xk = ("xt", i)
            sk = ("nst", t)
            b.stt(xt[i], xt[i], st_[:, t, 3:4], A, ALU.mult, ALU.mult, [xk, sk, "A"], [xk])
            b.tt("gpsimd", xt[i], xt[i], Bc, ALU.add, [xk, "Bc"], [xk])

        def n3(t):
            i = t % NBX
            xk, hk = ("xt", i), ("hb", t % 2)
            hbt = hb[t % 2]
            b.act(hbt, xt[i], AF.Copy, [xk], [hk])
            for q4 in range(4):
                pi = 4 + (t * 4 + q4) % 4
                for cc in range(4):
                    c = q4 * 4 + cc
                    b.tr(psb[pi][:, cc * 128:(cc + 1) * 128], hbt[:, c * 128:(c + 1) * 128], identb, [hk, "identb"], [PS(pi)])
                b.cp_rr(hT[:, q4 * 4:(q4 + 1) * 4, t * 128:(t + 1) * 128],
                        psb[pi][:, 0:512].rearrange("p (c n) -> p c n", c=4), [PS(pi)], [("hT", t)])

        def n4(t):
            i = t % NBX
            xk = ("xt", i)
            for q4 in range(4):
                pi = (t * 4 + q4) % 2
                for cc in range(4):
                    c = q4 * 4 + cc
                    b.tr(ps[pi][:, cc * 128:(cc + 1) * 128], xt[i][:, c * 128:(c + 1) * 128], identf, [xk, "identf"], [PS(pi)])
                b.cp_rr(hTf[:, q4 * 4:(q4 + 1) * 4, :], ps[pi][:, 0:512].rearrange("p (c n) -> p c n", c=4), [PS(pi)], [("hTf", q4)])
            for c in range(NC_):
                b.mm(ps[2][:, 0:16], hTf[:, c, :], wr[:, c, :], c == 0, c == NC_ - 1, [("hTf", c // 4), "wr"], [PS(2)])
            b.act(aff_all[:, t, :], ps[2][:, 0:16], AF.Sigmoid, [PS(2)], ["aff"])

        stages = [n1, n2, n3] + ([n4] if router is not None else [])
        for step in range(NT + len(stages) - 1):
            for si, stg in enumerate(stages):
                idx = step - si
                if 0 <= idx < NT:
                    stg(idx)
        if hts is not None:
            for c in range(NC_):
                b.dma("sync", hts[:, c, :], hT[:, c, :], [("hT", t) for t in range(NT)], [])

    import os
    SEL = os.environ.get("KSEL", "")

    def phase_inproj(l, hT):
        cosT = b.sb("cosT", [128, S_LEN], F32)
        sinT = b.sb("sinT", [128, S_LEN], F32)
        pswap = b.sb("pswap", [128, 128], BF16)
        b.dma("sync", cosT, k_cosT, [], ["cosT"])
        b.dma("sync", sinT, k_sinT, [], ["sinT"])
        b.dma("sync", pswap, k_pswap, [], ["pswap"])
        wblk = [b.sb("wblk%d" % i, [128, NC_, 512], BF16) for i in range(3)]
        ev = [b.sb("ev%d" % i, [128, 512], F32) for i in range(4)]
        evb = [b.sb("evb%d" % i, [128, 512], BF16) for i in range(4)]
        t1 = [b.sb("t1_%d" % i, [128, 512], F32) for i in range(2)]
        t2 = [b.sb("t2_%d" % i, [128, 512], F32) for i in range(2)]
        qsb = [b.sb("qsb%d" % i, [128, 512], BF16) for i in range(2)]
        wv = w_in[l].rearrange("(c p) n -> p c n", p=128)
        hTr = [("hT", t) for t in range(NT)]
        state = {"n": 0, "e": 0, "p": 0, "a": 0}
        ada_em = make_ada_emitter(l + 1, (6, 7)) if l + 1 < DEPTH else None

        def ada_hook():
            if ada_em is not None and state["a"] < 24:
                ada_em(state["a"])
                state["a"] += 1

        def load(col0, ncols=512):
            i = state["n"] % 3
            state["n"] += 1
            b.dma("gpsimd", wblk[i][:, :, 0:ncols], wv[:, :, col0:col0 + ncols], [], [("wblk", i)])
            return wblk[i], ("wblk", i)

        def nextps():
            state["p"] += 1
            return state["p"] % 4

        def fm_block(col0, nchunks, evac, width=128):
            wb, wk = load(col0, 512 if nchunks * width > 16 else 16)
            pend = None
            for cc in range(nchunks):
                for tb in range(4):
                    pi = nextps()
                    for k in range(NC_):
                        b.mm(ps[pi][0:width, :], wb[:, k, cc * width:(cc + 1) * width], hT[:, k, tb * 512:(tb + 1) * 512],
                             k == 0, k == NC_ - 1, [wk] + hTr[tb * 4:(tb + 1) * 4], [PS(pi)])
                    if pend is not None:
                        evac(*pend)
                    pend = (pi, cc, tb)
            evac(*pend)
            ada_hook()

        def tm_block(col0, evac):
            wb, wk = load(col0)
            pend = None
            for t in range(NT):
                pi = nextps()
                for k in range(NC_):
                    b.mm(ps[pi], hT[:, k, t * 128:(t + 1) * 128], wb[:, k, :], k == 0, k == NC_ - 1, [wk, hTr[t]], [PS(pi)])
                if pend is not None:
                    evac(*pend)
                pend = (pi, t)
            evac(*pend)
            ada_hook()

        def nextev():
            state["e"] += 1
            return state["e"] % 4

        def rope_evac(dst, head0):
            def f(pi, cc, tb):
                j = state["e"] % 2
                state["e"] += 1
                tok = slice(tb * 512, (tb + 1) * 512)
                b.act(qsb[j], ps[pi], AF.Copy, [PS(pi)], [("qsb", j)])
                p2 = 4 + j
                VAR = os.environ.get("KVAR", "")
                if VAR != "noswap":
                    b.mm(ps[p2], pswap, qsb[j], True, True, [("qsb", j), "pswap"], [PS(p2)])
                else:
                    p2 = pi
                b.tt("vector", t1[j], ps[pi], cosT[:, tok], ALU.mult, [PS(pi), "cosT", ("qsb", j)], [("t1", j)])
                b.tt("vector", t2[j], ps[p2], sinT[:, tok], ALU.mult, [PS(p2), "sinT"], [("t2", j)])
                b.tt("vector", evb[j], t1[j], t2[j], ALU.add, [("t1", j), ("t2", j)], [("evb", j)])
                b.dma("sync", dst[head0 + cc, :, tok], evb[j], [("evb", j)], [])
            return f

        def plain_fm(dst_fn, func, dt, width=128):
            def f(pi, cc, tb):
                j = nextev()
                tok = slice(tb * 512, (tb + 1) * 512)
                buf = ev[j] if dt == F32 else evb[j]
                key = ("ev", j) if dt == F32 else ("evb", j)
                if func is None:
                    b.cp_rr(buf[0:width, :], ps[pi][0:width, :], [PS(pi)], [key])
                else:
                    b.act(buf[0:width, :], ps[pi][0:width, :], func, [PS(pi)], [key])
                b.dma("sync", dst_fn(cc)[:, tok], buf[0:width, :], [key], [])
            return f

        def plain_tm(dst, col0, func, dt):
            def f(pi, t):
                j = nextev()
                buf = ev[j] if dt == F32 else evb[j]
                key = ("ev", j) if dt == F32 else ("evb", j)
                if func is None:
                    b.cp_rr(buf, ps[pi], [PS(pi)], [key])
                else:
                    b.act(buf, ps[pi], func, [PS(pi)], [key])
                b.dma("sync", dst[t * 128:(t + 1) * 128, col0:col0 + 512], buf, [key], [])
            return f

        def on(c):
            return (not SEL) or (c in SEL)
        if on("a"):
            for g in range(3):
                fm_block(O_QA + g * 512, 4, rope_evac(qaT, g * 4))
            for g in range(3):
                fm_block(O_KA + g * 512, 4, rope_evac(kaT, g * 4))
        if on("b"):
            for g in range(3):
                tm_block(O_VA + g * 512, plain_tm(va, g * 512, None, BF16))
        if on("c"):
            fm_block(O_QB, 4, plain_fm(lambda cc: qbT[cc], None, F32))
            fm_block(O_KB, 4, plain_fm(lambda cc: kbT[cc], None, F32))
            tm_block(O_KB, plain_tm(kbt, 0, None, F32))
        if on("d"):
            for j in range(2):
                tm_block(O_VB + j * 512, plain_tm(vb, j * 512, None, BF16))
            for j in range(2):
                tm_block(O_RB + j * 512, plain_tm(rb, j * 512, AF.Silu, F32))
        if on("e"):
            fm_block(O_AL, 1, plain_fm(lambda cc: alT, None, F32, width=16), width=16)
        if on("f"):
            for j in range(4):
                fm_block(O_GA + j * 512, 4, plain_fm(lambda cc, j=j: sga[j * 4 + cc], AF.Sigmoid, BF16))
            for j in range(4):
                fm_block(O_GB + j * 512, 4, plain_fm(lambda cc, j=j: sgb[j * 4 + cc], AF.Sigmoid, BF16))
        while ada_em is not None and state["a"] < 24:
            ada_hook()

    def phase_attn(l):
        maskA = b.sb("maskA", [128, 256], F32)
        b.dma("sync", maskA, k_maskA, [], ["maskA"])
        qT = [b.sb("qT%d" % i, [128, S_LEN], BF16) for i in range(2)]
        kT = [b.sb("kT%d" % i, [128, S_LEN], BF16) for i in range(2)]
        vp = [b.sb("vp%d" % i, [128, 16, 128], BF16) for i in range(2)]
        ob = [b.sb("ob%d" % i, [128, 16, 130], F32) for i in range(2)]
        NBA = 8
        sm = [b.sb("sm%d" % i, [128, 256], F32) for i in range(NBA)]
        pp = [b.sb("pp%d" % i, [128, 256], BF16) for i in range(NBA)]
        pT = [b.sb("pT%d" % i, [128, 256], BF16) for i in range(NBA)]
        negm = [b.sb("negm%d" % i, [128, 1], F32) for i in range(NBA)]
        scale = 128.0 ** -0.5
        def pipeline(stages, items):
            for step in range(len(items) + len(stages) - 1):
                for si, stg in enumerate(stages):
                    idx = step - si
                    if 0 <= idx < len(items):
                        stg(items[idx])

        def head_load(head):
            g = head // 4
            d = GROUPS[g][1]
            nb = 16 // d
            hi = head % 2
            b.dma("sync", qT[hi], qaT[head], [], [("qT", hi)])
            b.dma("sync", kT[hi], kaT[head], [], [("kT", hi)])
            vsrc = va[:, head * 128:(head + 1) * 128].rearrange("(n j r) c -> j r n c", n=nb, j=128, r=d)
            for r in range(d):
                b.dma("sync", vp[hi][:, r * nb:(r + 1) * nb, :], vsrc[:, r, :, :], [], [("vp", hi)])

        items = []
        for head in range(12):
            g = head // 4
            d = GROUPS[g][1]
            nb = 16 // d
            for r in range(d):
                for n in range(nb):
                    it = dict(head=head, g=g, hh=head % 4, d=d, nb=nb, r=r, n=n, bi=r * nb + n, hi=head % 2,
                              j=len(items) % NBA, seq=len(items) % 16)
                    it["nk"] = 256 if n > 0 else 128
                    it["w0"] = 0 if n > 0 else 128
                    items.append(it)

        def keys(it):
            j, hi = it["j"], it["hi"]
            return ("sm", j), ("pp", j), ("pT", j), ("negm", j), ("ob", hi, it["bi"]), ("qT", hi), ("kT", hi), ("vp", hi)

        def stA(it):
            d, n, r, hi, j, nk = it["d"], it["n"], it["r"], it["hi"], it["j"], it["nk"]
            if it["seq"] == 8 and it["head"] + 1 < 12:
                head_load(it["head"] + 1)
            smk, ppk, pTk, nmk, ok, qk, kk, vk = keys(it)

            def cols(ap_, s0, n_):
                return ap_[:, s0:s0 + (n_ - 1) * d + 1:d] if d > 1 else ap_[:, s0:s0 + n_]
            q0 = d * 128 * n + r
            qcols = cols(qT[hi], q0, 128)
            if n > 0:
                kcols = cols(kT[hi], d * 128 * (n - 1) + r, 256)
            else:
                kcols = cols(kT[hi], q0, 128)
            b.mm(ps[j][:, 0:nk], qcols, kcols, True, True, [qk, kk], [PS(j)])

        def stB(it):
            hi, j, nk, w0, bi = it["hi"], it["j"], it["nk"], it["w0"], it["bi"]
            smk, ppk, pTk, nmk, ok, qk, kk, vk = keys(it)
            b.stt(sm[j][:, 0:nk], ps[j][:, 0:nk], scale, maskA[:, w0:w0 + nk], ALU.mult, ALU.add, [PS(j), "maskA"], [smk])
            b.red(ob[hi][:, bi, 128:129], sm[j][:, 0:nk], ALU.max, [smk], [ok])
            b.ts("vector", negm[j], ob[hi][:, bi, 128:129], -1.0, None, ALU.mult, None, [ok], [nmk])

        def stC(it):
            hi, j, nk, bi = it["hi"], it["j"], it["nk"], it["bi"]
            smk, ppk, pTk, nmk, ok, qk, kk, vk = keys(it)
            b.act(pp[j][:, 0:nk], sm[j][:, 0:nk], AF.Exp, [smk, nmk], [ppk, ok], bias=negm[j], accum=ob[hi][:, bi, 129:130])

        def stD(it):
            j, nk = it["j"], it["nk"]
            smk, ppk, pTk, nmk, ok, qk, kk, vk = keys(it)
            for hlf in range(nk // 128):
                b.tr(psb[j][:, 768 + hlf * 128:768 + (hlf + 1) * 128], pp[j][:, hlf * 128:(hlf + 1) * 128], identb, [ppk, "identb"], [PS(j)])

        def stE(it):
            j, nk = it["j"], it["nk"]
            smk, ppk, pTk, nmk, ok, qk, kk, vk = keys(it)
            b.cp_rr(pT[j][:, 0:nk], psb[j][:, 768:768 + nk], [PS(j)], [pTk])

        def stF(it):
            hi, j, n, bi = it["hi"], it["j"], it["n"], it["bi"]
            smk, ppk, pTk, nmk, ok, qk, kk, vk = keys(it)
            if n > 0:
                b.mm(ps[j][:, 256:384], pT[j][:, 0:128], vp[hi][:, bi - 1, :], True, False, [pTk, vk], [PS(j)])
                b.mm(ps[j][:, 256:384], pT[j][:, 128:256], vp[hi][:, bi, :], False, True, [pTk, vk], [PS(j)])
            else:
                b.mm(ps[j][:, 256:384], pT[j][:, 0:128], vp[hi][:, bi, :], True, True, [pTk, vk], [PS(j)])

        def stG(it):
            hi, j, bi = it["hi"], it["j"], it["bi"]
            smk, ppk, pTk, nmk, ok, qk, kk, vk = keys(it)
            b.cp_rr(ob[hi][:, bi, 0:128], ps[j][:, 256:384], [PS(j)], [ok])
            if it["seq"] == 15:
                g, hh, d, nb = it["g"], it["hh"], it["d"], it["nb"]
                dst = oa[g][:, hh, :].rearrange("(n j r) c -> j r n c", n=nb, j=128, r=d)
                for r in range(d):
                    b.dma("sync", dst[:, r, :, :], ob[hi][:, r * nb:(r + 1) * nb, :], [("ob", hi, x_) for x_ in range(16)], [])

        head_load(0)
        pipeline([stA, stB, stC, stD, stE, stF, stG], items)
        S.barrier()
        og = [[b.sb("og%d_%d" % (i, g), [128, 4, 130], F32) for g in range(3)] for i in range(2)]
        M = [b.sb("M%d" % i, [128, 4], F32) for i in range(2)]
        eg = [b.sb("eg%d" % i, [128, 3, 4], F32) for i in range(2)]
        wd = [b.sb("wd%d" % i, [128, 4], F32) for i in range(2)]
        tmp = [b.sb("tmp%d" % i, [128, 4], F32) for i in range(2)]
        ya = [b.sb("ya%d" % i, [128, 4, 128], F32) for i in range(2)]
        yt = [b.sb("yt%d" % i, [128, 4, 128], F32) for i in range(2)]
        yab = [b.sb("yab%d" % i, [128, 512], BF16) for i in range(2)]
        yaT = b.sb("yaT", [128, 4, S_LEN], BF16)
        for t in range(NT):
            i = t % 2
            ogk = [("og", i, g) for g in range(3)]
            mk = ("mrg", i)
            for g in range(3):
                b.dma("sync", og[i][g], oa[g][t * 128:(t + 1) * 128], [], [ogk[g]])
            m = [og[i][g][:, :, 128] for g in range(3)]
            dn = [og[i][g][:, :, 129] for g in range(3)]
            b.tt("vector", M[i], m[0], m[1], ALU.max, [ogk[0], ogk[1]], [mk])
            b.tt("vector", M[i], M[i], m[2], ALU.max, [mk, ogk[2]], [mk])
            for g in range(3):
                b.tt("vector", eg[i][:, g, :], m[g], M[i], ALU.subtract, [ogk[g], mk], [mk])
            egf = eg[i].rearrange("p g h -> p (g h)")
            b.act(egf, egf, AF.Exp, [mk], [mk])
            b.tt("vector", wd[i], eg[i][:, 0, :], dn[0], ALU.mult, [mk, ogk[0]], [mk])
            for g in (1, 2):
                b.tt("vector", tmp[i], eg[i][:, g, :], dn[g], ALU.mult, [mk, ogk[g]], [mk])
                b.tt("vector", wd[i], wd[i], tmp[i], ALU.add, [mk], [mk])
            b.recip(wd[i], wd[i], [mk], [mk])
            for g in range(3):
                b.tt("vector", eg[i][:, g, :], eg[i][:, g, :], wd[i], ALU.mult, [mk], [mk])
            yk = ("ya", i)
            for g in range(3):
                cb = eg[i][:, g, :].unsqueeze(2).to_broadcast([128, 4, 128])
                dsto = ya[i] if g == 0 else yt[i]
                b.tt("vector", dsto, og[i][g][:, :, 0:128], cb, ALU.mult, [ogk[g], mk], [yk if g == 0 else ("yt", i)])
                if g > 0:
                    b.tt("gpsimd", ya[i], ya[i], yt[i], ALU.add, [yk, ("yt", i)], [yk])
            b.act(yab[i], ya[i].rearrange("p h c -> p (h c)"), AF.Copy, [yk], [("yab", i)])
            pi = 6 + i
            for hh in range(4):
                b.tr(psb[pi][:, hh * 128:(hh + 1) * 128], yab[i][:, hh * 128:(hh + 1) * 128], identb, [("yab", i), "identb"], [PS(pi)])
            b.cp_rr(yaT[:, :, t * 128:(t + 1) * 128], psb[pi][:, 0:512].rearrange("p (c n) -> p c n", c=4), [PS(pi)], [("yaT", t)])
        for hh in range(4):
            b.dma("sync", yaT_d[:, hh, :], yaT[:, hh, :], [("yaT", t_) for t_ in range(NT)], [])

    def phase_gla(l):
        Uf = b.sb("Uf", [128, 128], F32)
        Ub = b.sb("Ub", [128, 128], BF16)
        Lf = b.sb("Lf", [128, 128], F32)
        b.dma("sync", Uf, k_Uf, [], ["Uf"])
        b.dma("sync", Ub, k_Ub, [], ["Ub"])
        b.dma("sync", Lf, k_Lf, [], ["Lf"])
        wa2 = b.sb("wa2", [16, 512], F32)
        ba2 = b.sb("ba2", [1, 512], F32)
        ones1 = b.sb("ones1", [1, 128], F32)
        gain = b.sb("gain", [128, 1024], F32)
        alow = b.sb("alow", [16, S_LEN], F32)
        b.dma("sync", wa2, w_alpha2[l], [], ["wa2"])
        b.dma("sync", ba2, b_alpha2[l:l + 1, :], [], ["ba2"])
        b.memset("vector", ones1, 1.0, [], ["ones1"])
        b.dma("sync", gain, gla_gain[l].partition_broadcast(128), [], ["gain"])
        b.dma("sync", alow, alT, [], ["alow"])
        Sst = [b.sb("Sst%d" % h, [128, 256], F32) for h in range(4)]
        Sbf = [b.sb("Sbf%d" % h, [128, 256], BF16) for h in range(4)]
        for h in range(4):
            b.memset("vector", Sst[h], 0.0, [], [("Sst", h)])
            b.memset("gpsimd", Sbf[h], 0.0, [], [("Sbf", h)])
        ybT = b.sb("ybT", [128, 8, S_LEN], BF16)
        NB = 3
        qf = [b.sb("qf%d" % i, [128, 4, 128], F32) for i in range(NB)]
        kf = [b.sb("kf%d" % i, [128, 4, 128], F32) for i in range(NB)]
        kt = [b.sb("kt%d" % i, [128, 512], F32) for i in range(NB)]
        vt = [b.sb("vt%d" % i, [128, 1024], BF16) for i in range(NB)]
        rt = [b.sb("rt%d" % i, [128, 1024], F32) for i in range(NB)]
        ee = [b.sb("ee%d" % i, [128, 512], F32) for i in range(NB)]
        sp = [b.sb("sp%d" % i, [128, 512], F32) for i in range(NB)]
        NBG = 4
        eb = [b.sb("eb%d" % i, [128, 128], F32) for i in range(NBG)]
        ebi = [b.sb("ebi%d" % i, [128, 128], F32) for i in range(NBG)]
        erv = [b.sb("erv%d" % i, [128, 128], F32) for i in range(NBG)]
        qd = [b.sb("qd%d" % i, [128, 128], BF16) for i in range(NBG)]
        ki = [b.sb("ki%d" % i, [128, 128], BF16) for i in range(NBG)]
        ke = [b.sb("ke%d" % i, [128, 128], BF16) for i in range(NBG)]
        am = [b.sb("am%d" % i, [128, 128], BF16) for i in range(NBG)]
        osb = [b.sb("osb%d" % i, [128, 256], F32) for i in range(NBG)]
        oj = [b.sb("oj%d" % i, [128, 256], F32) for i in range(NBG)]
        gs = [b.sb("gs%d" % i, [128, 4], F32) for i in range(NBG)]
        yb = [b.sb("yb%d" % i, [128, 256], BF16) for i in range(NBG)]

        def prologue(t):
            i = t % NB
            tok = slice(t * 128, (t + 1) * 128)
            b.dma("sync", qf[i], qbT[:, :, tok].rearrange("h p n -> p h n"), [], [("qf", i)])
            b.dma("sync", kf[i], kbT[:, :, tok].rearrange("h p n -> p h n"), [], [("kf", i)])
            b.dma("sync", kt[i], kbt[tok, :], [], [("kt", i)])
            b.dma("sync", vt[i], vb[tok, :], [], [("vt", i)])
            b.dma("sync", rt[i], rb[tok, :], [], [("rt", i)])
            pz = 1
            b.mm(ps[pz], alow[:, tok], wa2, True, False, ["alow", "wa2"], [PS(pz)])
            b.mm(ps[pz], ones1, ba2, False, True, ["ones1", "ba2"], [PS(pz)])
            b.act(ee[i], ps[pz], AF.Exp, [PS(pz)], [("ee", i)], scale=-1.0)
            b.act(sp[i], ee[i], AF.Ln, [("ee", i)], [("sp", i)], bias=1.0)

        units = []
        for t in range(NT):
            for h in range(4):
                units.append(dict(t=t, h=h, i=t % NB, j=len(units) % NBG, tok=slice(t * 128, (t + 1) * 128)))

        def g1(u):
            t, h, i, j = u["t"], u["h"], u["i"], u["j"]
            bA = 2 * j
            hc = slice(h * 128, (h + 1) * 128)
            spk = ("sp", i)
            b.mm(ps[bA][:, 0:128], sp[i][:, hc], Uf, True, True, [spk, "Uf"], [PS(bA)])
            b.mm(ps[bA][:, 128:256], Lf, sp[i][:, hc], True, True, [spk, "Lf"], [PS(bA)])
            ek = ("eb", j)
            b.act(eb[j], ps[bA][:, 0:128], AF.Exp, [PS(bA)], [ek], scale=-1.0 / 16)
            b.act(ebi[j], ps[bA][:, 0:128], AF.Exp, [PS(bA)], [("ebi", j)], scale=1.0 / 16)
            b.act(erv[j], ps[bA][:, 128:256], AF.Exp, [PS(bA)], [("erv", j)], scale=-1.0 / 16)
            b.stt(qd[j], qf[i][:, h, :], 128.0 ** -0.5, eb[j], ALU.mult, ALU.mult, [("qf", i), ek], [("qd", j)])
            b.tt("gpsimd", ki[j], kf[i][:, h, :], ebi[j], ALU.mult, [("kf", i), ("ebi", j)], [("ki", j)])
            b.tt("gpsimd", ke[j], kt[i][:, hc], erv[j], ALU.mult, [("kt", i), ("erv", j)], [("ke", j)])
            if h == 0 and t + 1 < NT:
                prologue(t + 1)

        def g2(u):
            j = u["j"]
            bA = 2 * j
            b.mm(ps[bA][:, 256:384], ki[j], qd[j], True, True, [("ki", j), ("qd", j)], [PS(bA)])
            b.tt("vector", am[j], ps[bA][:, 256:384], Ub, ALU.mult, [PS(bA), "Ub"], [("am", j)])

        def g3(u):
            t, h, i, j = u["t"], u["h"], u["i"], u["j"]
            bB = 2 * j + 1
            ek = ("eb", j)
            vh = vt[i][:, h * 256:(h + 1) * 256]
            b.mm(ps[bB][:, 0:256], am[j], vh, True, False, [("am", j), ("vt", i)], [PS(bB)])
            b.mm(ps[bB][:, 0:256], qd[j], Sbf[h], False, True, [("qd", j), ("Sbf", h)], [PS(bB)])
            b.mm(ps[bB][:, 256:512], ke[j], vh, True, True, [("ke", j), ("vt", i)], [PS(bB)])
            b.stt(Sst[h], Sst[h], eb[j][:, 127:128], ps[bB][:, 256:512], ALU.mult, ALU.add, [("Sst", h), ek, PS(bB)], [("Sst", h)])
            b.act(Sbf[h], Sst[h], AF.Copy, [("Sst", h)], [("Sbf", h)])
            b.act(osb[j], ps[bB][:, 0:256], AF.Copy, [PS(bB)], [("osb", j)])

        def g4(u):
            t, h, i, j, tok = u["t"], u["h"], u["i"], u["j"], u["tok"]
            bA = 2 * j
            ok_ = ("osb", j)
            gk = ("gs", j)
            b.stt(oj[j], osb[j], 1.0, osb[j], ALU.mult, ALU.mult, [ok_], [("oj", j), gk], accum=gs[j][:, 0:1])
            b.ts("vector", gs[j][:, 1:2], gs[j][:, 0:1], 1.0 / 256, EPS, ALU.mult, ALU.add, [gk], [gk])
            b.act(gs[j][:, 2:3], gs[j][:, 1:2], AF.Sqrt, [gk], [gk])
            b.recip(gs[j][:, 3:4], gs[j][:, 2:3], [gk], [gk])
            b.stt(oj[j], osb[j], gs[j][:, 3:4], gain[:, h * 256:(h + 1) * 256], ALU.mult, ALU.mult, [ok_, gk, "gain"], [("oj", j)])
            b.tt("gpsimd", yb[j], oj[j], rt[i][:, h * 256:(h + 1) * 256], ALU.mult, [("oj", j), ("rt", i)], [("yb", j)])
            for c2 in range(2):
                b.tr(psb[bA][:, 768 + c2 * 128:768 + (c2 + 1) * 128], yb[j][:, c2 * 128:(c2 + 1) * 128], identb, [("yb", j), "identb"], [PS(bA)])
            b.cp_rr(ybT[:, h * 2:(h + 1) * 2, tok], psb[bA][:, 768:1024].rearrange("p (c n) -> p c n", c=2), [PS(bA)], [("ybT", t, h)])

        prologue(0)
        for step in range(len(units) + 3):
            for si, stg in enumerate((g1, g2, g3, g4)):
                idx = step - si
                if 0 <= idx < len(units):
                    stg(units[idx])
        for c in range(8):
            b.dma("sync", ybT_d[:, c, :], ybT[:, c, :], [("ybT", t_, c // 2) for t_ in range(NT)], [])

    def phase_out(l, x_src, x_dst):
        mT = b.sb("mT", [128, NC_, S_LEN], BF16)
        mark_o = b.sb_off
        yaT = b.sb("yaT", [128, 4, S_LEN], BF16)
        ybT = b.sb("ybT", [128, 8, S_LEN], BF16)
        for hh in range(4):
            b.dma("sync", yaT[:, hh, :], yaT_d[:, hh, :], [], ["yaT"])
        for c in range(8):
            b.dma("sync", ybT[:, c, :], ybT_d[:, c, :], [], ["ybT"])
        woa = b.sb("woa", [128, 4, D], BF16)
        wob = b.sb("wob", [128, 8, D], BF16)
        b.dma("gpsimd", woa, w_out_a[l].rearrange("(c p) n -> p c n", p=128), [], ["woa"])
        for c in range(8):
            b.dma("gpsimd", wob[:, c, :], w_out_b[l, c * 128:(c + 1) * 128, :], [], ["wob"])
        ga = [b.sb("ga%d" % i, [128, 512], BF16) for i in range(2)]
        gb = [b.sb("gb%d" % i, [128, 512], BF16) for i in range(2)]
        m1 = [b.sb("m1_%d" % i, [128, 512], F32) for i in range(2)]
        m2 = [b.sb("m2_%d" % i, [128, 512], F32) for i in range(2)]
        n = 0
        for dc in range(NC_):
            for tb in range(4):
                j = n % 2
                n += 1
                tok = slice(tb * 512, (tb + 1) * 512)
                b.dma("sync", ga[j], sga[dc, :, tok], [], [("ga", j)])
                b.dma("sync", gb[j], sgb[dc, :, tok], [], [("gb", j)])
                pa, pb = j, 2 + j
                for k in range(4):
                    b.mm(ps[pa], woa[:, k, dc * 128:(dc + 1) * 128], yaT[:, k, tok], k == 0, k == 3, ["woa", "yaT"], [PS(pa)])
                for k in range(8):
                    b.mm(ps[pb], wob[:, k, dc * 128:(dc + 1) * 128], ybT[:, k, tok], k == 0, k == 7, ["wob", "ybT"], [PS(pb)])
                b.tt("vector", m1[j], ps[pa], ga[j], ALU.mult, [PS(pa), ("ga", j)], [("m1", j)])
                b.tt("vector", m2[j], ps[pb], gb[j], ALU.mult, [PS(pb), ("gb", j)], [("m2", j)])
                b.tt("gpsimd", mT[:, dc, tok], m1[j], m2[j], ALU.add, [("m1", j), ("m2", j)], [("mT", tb)])
        S.barrier()
        b.sb_off = mark_o
        gate_bc = b.sb("gate_bc", [128, D], F32)
        b.dma("sync", gate_bc, ada[l, 2 * D:3 * D].partition_broadcast(128), [], ["gate_bc"])
        wo = [b.sb("wo%d" % i, [128, NC_, 512], BF16) for i in range(2)]
        xt = [b.sb("xo%d" % i, [128, 512], F32) for i in range(3)]
        xg = [b.sb("xg%d" % i, [128, 512], F32) for i in range(3)]
        wv = w_out[l].rearrange("(c p) n -> p c n", p=128)
        n = 0
        for db in range(4):
            wj = db % 2
            dsl = slice(db * 512, (db + 1) * 512)
            b.dma("gpsimd", wo[wj], wv[:, :, dsl], [], [("wo", wj)])
            for t in range(NT):
                j = n % 3
                pi = 4 + n % 4
                n += 1
                b.dma("sync", xt[j], x_src[t * 128:(t + 1) * 128, dsl], [], [("xo", j)])
                for k in range(NC_):
                    b.mm(ps[pi], mT[:, k, t * 128:(t + 1) * 128], wo[wj][:, k, :], k == 0, k == NC_ - 1, [("wo", wj)], [PS(pi)])
                b.tt("vector", xg[j], ps[pi], gate_bc[:, dsl], ALU.mult, [PS(pi), "gate_bc"], [("xg", j)])
                b.tt("gpsimd", xg[j], xg[j], xt[j], ALU.add, [("xg", j), ("xo", j)], [("xg", j)])
                b.dma("sync", x_dst[t * 128:(t + 1) * 128, dsl], xg[j], [("xg", j)], [])

    def phase_route(aff_all, gates):
        brt = b.sb("brt", [128, 16], F32)
        b.dma("sync", brt, b_router.partition_broadcast(128), [], ["brt"])
        sel = b.sb("sel", [128, NT, 16], F32)
        ps_ = b.sb("pairs", [128, NT, 4, 6], F32)
        gsc = b.sb("gsc", [128, NT, 4], F32)
        m1 = b.sb("rm1", [128, NT, 4], F32)
        m2 = b.sb("rm2", [128, NT, 4], F32)
        gmx = b.sb("gmx", [128, NT], F32)
        gmask = b.sb("gmask", [128, NT, 4], F32)
        emask = b.sb("emask", [128, NT, 16], F32)
        den = b.sb("rden", [128, NT], F32)
        k = "route"
        b.tt("vector", sel, aff_all, brt.unsqueeze(1).to_broadcast([128, NT, 16]), ALU.add, ["aff", "brt"], [k])
        s4 = sel.rearrange("p t (g j) -> p t g j", g=4)
        pairs = [(0, 1), (0, 2), (0, 3), (1, 2), (1, 3), (2, 3)]
        for pi, (a_, c_) in enumerate(pairs):
            b.tt("vector", ps_[:, :, :, pi], s4[:, :, :, a_], s4[:, :, :, c_], ALU.add, [k], [k])
        b.S.op("vector", lambda e: e.tensor_reduce(out=gsc, in_=ps_, axis=AX.X, op=ALU.max), [k], [k])
        b.S.op("vector", lambda e: e.tensor_reduce(out=m1, in_=s4, axis=AX.X, op=ALU.max), [k], [k])
        pm_ = b.sb("pmins", [128, NT, 4, 6], F32)
        for pi, (a_, c_) in enumerate(pairs):
            b.tt("vector", pm_[:, :, :, pi], s4[:, :, :, a_], s4[:, :, :, c_], ALU.min, [k], [k])
        b.S.op("vector", lambda e: e.tensor_reduce(out=m2, in_=pm_, axis=AX.X, op=ALU.max), [k], [k])
        b.S.op("vector", lambda e: e.tensor_reduce(out=gmx, in_=gsc, axis=AX.X, op=ALU.max), [k], [k])
        b.tt("vector", gmask, gsc, gmx.unsqueeze(2).to_broadcast([128, NT, 4]), ALU.is_ge, [k], [k])
        e4 = emask.rearrange("p t (g j) -> p t g j", g=4)
        b.tt("vector", e4, s4, m2.unsqueeze(3).to_broadcast([128, NT, 4, 4]), ALU.is_ge, [k], [k])
        b.tt("vector", e4, e4, gmask.unsqueeze(3).to_broadcast([128, NT, 4, 4]), ALU.mult, [k], [k])
        b.tt("vector", gates, emask, aff_all, ALU.mult, [k, "aff"], ["gates"])
        b.S.op("vector", lambda e: e.tensor_reduce(out=den, in_=gates, axis=AX.X, op=ALU.add), ["gates"], [k])
        b.recip(den, den, [k], [k])
        b.tt("vector", gates, gates, den.unsqueeze(2).to_broadcast([128, NT, 16]), ALU.mult, ["gates", k], ["gates"])
        b.dma("sync", gates_d, gates, ["gates"], [])

    def phase_moe(l, x_src, x_dst, gates, final=None):
        hTh = b.sb("hTh", [128, NC_, 1024], BF16)
        yacc = b.sb("yacc", [128, 8, D], F32)
        aT = [b.sb("aT%d" % i, [128, 8, 1024], BF16) for i in range(2)]
        wg = [b.sb("wg%d" % i, [128, NC_, 256], BF16) for i in range(2)]
        wu = [b.sb("wu%d" % i, [128, NC_, 256], BF16) for i in range(2)]
        wd = [b.sb("wd%d" % i, [128, 8, 512], BF16) for i in range(2)]
        sg = [b.sb("sg%d" % i, [128, 512], F32) for i in range(2)]
        gate_bc = b.sb("gate2_bc", [128, D], F32)
        b.dma("sync", gate_bc, ada[l, 5 * D:6 * D].partition_broadcast(128), [], ["gate_bc"])
        xq = [b.sb("xq%d" % i, [128, 512], F32) for i in range(2)]
        if final is not None:
            fn_bc = b.sb("fn_bc", [128, D], F32)
            b.dma("sync", fn_bc, final_norm.partition_broadcast(128), [], ["fn_bc"])
            fst = b.sb("fst", [128, 8, 4], F32)
            fjunk = b.sb("fjunk", [128, D], BF16)
        nw = 0
        nd = 0
        ns = 0
        np_ = 0
        def load_hTh(half_):
            for c in range(NC_):
                b.dma("sync", hTh[:, c, :], hTs[:, c, half_ * 1024:(half_ + 1) * 1024], [], ["hTh"])

        load_hTh(0)
        for half in range(2):
            for e in range(16):
                ai = e % 2
                ak = ("aT", ai)
                wgv = w_gate_e[l, e].rearrange("(c p) n -> p c n", p=128)
                wuv = w_up_e[l, e].rearrange("(c p) n -> p c n", p=128)
                for fb in range(4):
                    wi = nw % 2
                    nw += 1
                    b.dma("gpsimd", wg[wi], wgv[:, :, fb * 256:(fb + 1) * 256], [], [("wg", wi)])
                    b.dma("gpsimd", wu[wi], wuv[:, :, fb * 256:(fb + 1) * 256], [], [("wu", wi)])
                    for cc in range(2):
                        fch = fb * 2 + cc
                        for tb in range(2):
                            tok = slice(tb * 512, (tb + 1) * 512)
                            pg = (np_ % 2) * 2
                            pu = pg + 1
                            np_ += 1
                            for k in range(NC_):
                                b.mm(ps[pg], wg[wi][:, k, cc * 128:(cc + 1) * 128], hTh[:, k, tok], k == 0, k == NC_ - 1, [("wg", wi), "hTh"], [PS(pg)])
                            for k in range(NC_):
                                b.mm(ps[pu], wu[wi][:, k, cc * 128:(cc + 1) * 128], hTh[:, k, tok], k == 0, k == NC_ - 1, [("wu", wi), "hTh"], [PS(pu)])
                            si = ns % 2
                            ns += 1
                            b.act(sg[si], ps[pg], AF.Silu, [PS(pg)], [("sg", si)])
                            b.tt("vector", aT[ai][:, fch, tok], ps[pu], sg[si], ALU.mult, [PS(pu), ("sg", si)], [ak])
                wdv = w_down_e[l, e].rearrange("(c p) n -> p c n", p=128)
                for db in range(4):
                    di = nd % 2
                    nd += 1
                    dsl = slice(db * 512, (db + 1) * 512)
                    b.dma("gpsimd", wd[di], wdv[:, :, dsl], [], [("wd", di)])
                    for t8 in range(8):
                        t = half * 8 + t8
                        py = 4 + np_ % 4
                        np_ += 1
                        for f in range(8):
                            b.mm(ps[py], aT[ai][:, f, t8 * 128:(t8 + 1) * 128], wd[di][:, f, :], f == 0, f == 7, [ak, ("wd", di)], [PS(py)])
                        yk = ("yacc", t8, db)
                        if e == 0:
                            b.ts("vector", yacc[:, t8, dsl], ps[py], gates[:, t, e:e + 1], None, ALU.mult, None, [PS(py), "gates"], [yk])
                        else:
                            b.stt(yacc[:, t8, dsl], ps[py], gates[:, t, e:e + 1], yacc[:, t8, dsl], ALU.mult, ALU.add, [PS(py), "gates", yk], [yk])
            if half == 0:
                load_hTh(1)
            for t8 in range(8):
                t = half * 8 + t8
                for db in range(4):
                    dsl = slice(db * 512, (db + 1) * 512)
                    j = (t8 * 4 + db) % 2
                    yk = ("yacc", t8, db)
                    b.dma("sync", xq[j], x_src[t * 128:(t + 1) * 128, dsl], [], [("xq", j)])
                    b.tt("vector", yacc[:, t8, dsl], yacc[:, t8, dsl], gate_bc[:, dsl], ALU.mult, [yk, "gate_bc"], [yk])
                    b.tt("gpsimd", yacc[:, t8, dsl], yacc[:, t8, dsl], xq[j], ALU.add, [yk, ("xq", j)], [yk])
                    if final is None:
                        b.dma("sync", x_dst[t * 128:(t + 1) * 128, dsl], yacc[:, t8, dsl], [yk], [])
                if final is not None:
                    yks = [("yacc", t8, db) for db in range(4)]
                    fk = ("fst", t8)
                    b.act(fjunk, yacc[:, t8, :], AF.Square, yks, ["fjunk", fk], accum=fst[:, t8, 0:1])
                    b.ts("vector", fst[:, t8, 1:2], fst[:, t8, 0:1], 1.0 / D, EPS, ALU.mult, ALU.add, [fk], [fk])
                    b.act(fst[:, t8, 2:3], fst[:, t8, 1:2], AF.Sqrt, [fk], [fk])
                    b.recip(fst[:, t8, 3:4], fst[:, t8, 2:3], [fk], [fk])
                    b.stt(yacc[:, t8, :], yacc[:, t8, :], fst[:, t8, 3:4], fn_bc, ALU.mult, ALU.mult, yks + [fk, "fn_bc"], yks)
                    b.dma("sync", final[t * 128:(t + 1) * 128, :], yacc[:, t8, :], yks, [])

    phase_ada()
    if upto >= 1:
        for l in range(DEPTH):
            xa, xb_, xc = xs[2 * l], xs[2 * l + 1], xs[2 * l + 2]
            b.sb_off = persist_end
            hT = b.sb("hT", [128, NC_, S_LEN], BF16)
            mark = b.sb_off
            phase_norm(l, xa, norm_mix[l], 0, 1, hT, hts=(hTs if "hTs" in b.debug else None))
            S.barrier()
            if upto >= 2:
                b.sb_off = mark
                phase_inproj(l, hT)
                S.barrier()
            if upto >= 3:
                b.sb_off = persist_end
                phase_attn(l)
                S.barrier()
            if upto >= 4:
                b.sb_off = persist_end
                phase_gla(l)
                S.barrier()
            if upto >= 5:
                b.sb_off = persist_end
                phase_out(l, xa, xb_)
                S.barrier()
            if upto >= 6:
                b.sb_off = persist_end
                gates = gates_p
                aff_all = b.sb("aff_all", [128, NT, 16], F32)
                wr = b.sb("wr", [128, NC_, 16], F32)
                hTf = b.sb("hTf", [128, NC_, 128], F32)
                b.dma("sync", wr, w_router.rearrange("(c p) e -> p c e", p=128), [], ["wr"])
                hT = b.sb("hT", [128, NC_, S_LEN], BF16)
                mark2 = b.sb_off
                phase_norm(l, xb_, norm_ffn[l], 3, 4, hT, router=(wr, aff_all, hTf), hts=hTs)
                S.barrier()
                b.sb_off = mark2
                phase_route(aff_all, gates)
                S.barrier()
            if upto >= 7:
                b.sb_off = persist_end
                gates = gates_p
                last = (l == DEPTH - 1)
                phase_moe(l, xb_, xc, gates, final=(out if last else None))
                S.barrier()
            if upto < 7 and l == 0:
                break
    if upto < 7:
        b.sb_off = persist_end
        z = b.sb("z", [128, D], F32)
        b.memset("vector", z, 0.0, [], ["z"])
        for t in range(NT):
            b.dma("sync", out[t * 128:(t + 1) * 128, :], z, ["z"], ["out"])
    S.finish()
    S.emit()
    st.close()
    return nc


_CONSTS = None


def kernel(**inputs):
    global _CONSTS
    if _CONSTS is None:
        _CONSTS = host_consts()
    nc = build()
    shared = {k: np.ascontiguousarray(v) for k, v in inputs.items() if k not in ("x", "c")}
    shared.update(_CONSTS)
    in_maps = []
    for i in range(8):
        m = dict(shared)
        m["x"] = np.ascontiguousarray(inputs["x"][i])
        m["c"] = np.ascontiguousarray(inputs["c"][i])
        in_maps.append(m)
    res = run_bass_kernel_spmd(nc, in_maps, core_ids=list(range(8)))
    return np.stack([r["out"] for r in res.results], axis=0).astype(np.float32)
```

```python
import contextlib
import numpy as np
import concourse.bass as bass
import concourse.mybir as mybir
from concourse.bass_utils import run_bass_kernel_spmd

F32 = mybir.dt.float32
BF16 = mybir.dt.bfloat16
AF = mybir.ActivationFunctionType
ALU = mybir.AluOpType
AX = mybir.AxisListType

D = 2048
S_LEN = 2048
NT = 16
NC_ = 16
DEPTH = 2
IN_W = 11792
EPS = 1e-6
NEG = -1.0e9
O_QA, O_KA, O_VA, O_QB, O_KB, O_VB, O_RB, O_AL, O_GA, O_GB = 0, 1536, 3072, 4608, 5120, 5632, 6656, 7680, 7696, 9744
GROUPS = ((128, 1), (512, 4), (2048, 16))


class Op:
    __slots__ = ("eng", "fn", "waits", "signal", "dma_sem", "dma_val", "idx")

    def __init__(self, eng, fn):
        self.eng = eng
        self.fn = fn
        self.waits = []
        self.signal = False
        self.dma_sem = None
        self.dma_val = 0
        self.idx = 0


class Sched:
    def __init__(self, nc, n_dma_sems=48, same_engine_sync=True, n_sw=12):
        self.nc = nc
        self.ops = {e: [] for e in ("tensor", "vector", "scalar", "gpsimd", "sync")}
        self.last_writer = {}
        self.readers = {}
        self.n_dma_sems = n_dma_sems
        self.dma_count = [0] * n_dma_sems
        self.dma_last_tok = [None] * n_dma_sems
        self.dma_rr = 0
        self.n_sw = n_sw
        self.sw_rr = 0
        self.same_engine_sync = same_engine_sync
        self.barrier_toks = []
        self.last_compute = {}

    def _deps(self, reads, writes):
        toks = []
        for r in reads:
            w = self.last_writer.get(r)
            if w is not None:
                toks.append(w)
            if isinstance(r, tuple) and r[0] == "ps":
                toks.extend(self.readers.get(r, ()))
        for w_ in writes:
            w = self.last_writer.get(w_)
            if w is not None:
                toks.append(w)
            toks.extend(self.readers.get(w_, ()))
        return toks

    def _register(self, tok, reads, writes):
        for r in reads:
            self.readers.setdefault(r, []).append(tok)
        for w in writes:
            self.last_writer[w] = tok
            self.readers[w] = []

    def op(self, eng, fn, reads=(), writes=()):
        o = Op(eng, fn)
        o.idx = len(self.ops[eng])
        tok = ("c", eng, o.idx)
        for t in self._deps(reads, writes) + self.barrier_toks:
            if t[0] == "c" and t[1] == eng:
                if eng == "tensor" or not self.same_engine_sync:
                    continue
            o.waits.append(t)
        self.ops[eng].append(o)
        self.last_compute[eng] = tok
        self._register(tok, reads, writes)
        return tok

    def dma(self, queue, fn, reads=(), writes=()):
        o = Op(queue, fn)
        o.idx = len(self.ops[queue])
        if queue == "gpsimd":
            s = self.sw_rr
            self.sw_rr = (self.sw_rr + 1) % self.n_sw
        else:
            s = self.n_sw + self.dma_rr
            self.dma_rr = (self.dma_rr + 1) % (self.n_dma_sems - self.n_sw)
        prev = self.dma_last_tok[s]
        if prev is not None:
            o.waits.append(prev)
        self.dma_count[s] += 1
        o.dma_sem = s
        o.dma_val = 16 * self.dma_count[s]
        tok = ("d", s, o.dma_val)
        self.dma_last_tok[s] = tok
        o.waits.extend(self._deps(reads, writes))
        o.waits.extend(self.barrier_toks)
        self.ops[queue].append(o)
        self._register(tok, reads, writes)
        return tok

    def barrier(self):
        toks = [t for t in self.dma_last_tok if t is not None]
        toks.extend(self.last_compute.values())
        self.barrier_toks = toks
        self.last_writer = {}
        self.readers = {}

    def finish(self, eng="sync"):
        o = Op(eng, None)
        o.idx = len(self.ops[eng])
        o.waits = [t for t in self.dma_last_tok if t is not None]
        self.ops[eng].append(o)

    def emit(self):
        nc = self.nc
        for e, ops in self.ops.items():
            for o in ops:
                for t in o.waits:
                    if t[0] == "c":
                        self.ops[t[1]][t[2]].signal = True
        rank = {}
        for e, ops in self.ops.items():
            r = 0
            for o in ops:
                if o.signal and o.dma_sem is None:
                    r += 1
                    rank[(e, o.idx)] = r
        with contextlib.ExitStack() as st:
            esem = {e: st.enter_context(nc.semaphore("s_" + e)) for e in self.ops}
            dsem = [st.enter_context(nc.semaphore("d%d" % i)) for i in range(self.n_dma_sems)]
            block = st.enter_context(nc.Block())

            def body_for(e):
                ops = self.ops[e]

                def body(eng):
                    seen = {}
                    for o in ops:
                        need = {}
                        for t in o.waits:
                            if t[0] == "c":
                                key = ("c", t[1])
                                val = rank[(t[1], t[2])]
                            else:
                                key = ("d", t[1])
                                val = t[2]
                            if seen.get(key, 0) >= val:
                                continue
                            if need.get(key, 0) < val:
                                need[key] = val
                        for key, val in need.items():
                            sem = esem[key[1]] if key[0] == "c" else dsem[key[1]]
                            eng.wait_ge(sem, val)
                            seen[key] = val
                        if o.fn is None:
                            continue
                        inst = o.fn(eng)
                        if o.dma_sem is not None:
                            inst.then_inc(dsem[o.dma_sem], 16)
                        elif o.signal:
                            inst.then_inc(esem[e], 1)
                return body

            for e in self.ops:
                if self.ops[e]:
                    getattr(block, e)(body_for(e))


class B:
    def __init__(self, debug=(), upto=99):
        self.nc = bass.Bass("TRN2", target_bir_lowering=False)
        self.S = Sched(self.nc)
        self.debug = set(debug)
        self.upto = upto
        self.sb_off = 16384
        self.uid = 0
        self.rr = 0

    def dram_in(self, name, shape, dt=F32):
        return self.nc.dram_tensor(name, list(shape), dt, kind="ExternalInput").ap()

    def dram_out(self, name, shape, dt=F32):
        return self.nc.dram_tensor(name, list(shape), dt, kind="ExternalOutput").ap()

    def scratch(self, name, shape, dt=F32):
        kind = "ExternalOutput" if name in self.debug else "Internal"
        return self.nc.dram_tensor(name, list(shape), dt, kind=kind).ap()

    def sb(self, name, shape, dt):
        n = 1
        for s in shape[1:]:
            n *= s
        nbytes = n * (4 if dt == F32 else 2)
        off = (self.sb_off + 63) // 64 * 64
        self.uid += 1
        t = self.nc.alloc_sbuf_tensor_at("%s_%d" % (name, self.uid), list(shape), dt, offset=off)
        self.sb_off = off + nbytes
        assert self.sb_off <= 16384 + 212000, (name, self.sb_off)
        return t.ap()

    def mm(self, out, lhsT, rhs, start, stop, r, w):
        self.S.op("tensor", lambda e: e.matmul(out, lhsT=lhsT, rhs=rhs, start=start, stop=stop), r, w)

    def tr(self, out, in_, ident, r, w):
        self.S.op("tensor", lambda e: e.transpose(out=out, in_=in_, identity=ident), r, w)

    def act(self, out, in_, func, r, w, bias=None, scale=1.0, accum=None):
        def f(e):
            kw = {}
            if bias is not None:
                kw["bias"] = bias
            if accum is not None:
                kw["accum_out"] = accum
            return e.activation(out=out, in_=in_, func=func, scale=scale, **kw)
        self.S.op("scalar", f, r, w)

    def tt(self, eng, out, in0, in1, op, r, w):
        self.S.op(eng, lambda e: e.tensor_tensor(out=out, in0=in0, in1=in1, op=op), r, w)

    def ts(self, eng, out, in0, s1, s2, op0, op1, r, w):
        if op1 is None:
            self.S.op(eng, lambda e: e.tensor_scalar(out=out, in0=in0, scalar1=s1, scalar2=None, op0=op0), r, w)
        else:
            self.S.op(eng, lambda e: e.tensor_scalar(out=out, in0=in0, scalar1=s1, scalar2=s2, op0=op0, op1=op1), r, w)

    def stt(self, out, in0, scalar, in1, op0, op1, r, w, accum=None):
        def f(e):
            if accum is not None:
                return e.scalar_tensor_tensor(out=out, in0=in0, scalar=scalar, in1=in1, op0=op0, op1=op1, accum_out=accum)
            return e.scalar_tensor_tensor(out=out, in0=in0, scalar=scalar, in1=in1, op0=op0, op1=op1)
        self.S.op("vector", f, r, w)

    def cp(self, eng, out, in_, r, w):
        if eng == "scalar":
            self.act(out, in_, AF.Copy, r, w)
        else:
            self.S.op(eng, lambda e: e.tensor_copy(out=out, in_=in_), r, w)

    def cp_rr(self, out, in_, r, w):
        self.rr += 1
        self.cp("scalar" if self.rr % 2 else "vector", out, in_, r, w)

    def dma(self, q, out, in_, r, w, slow=False):
        if slow:
            self.S.dma(q, lambda e: e.dma_start(out=out, in_=in_, allow_slow_non_contiguous=True), r, w)
        else:
            self.S.dma(q, lambda e: e.dma_start(out=out, in_=in_), r, w)

    def red(self, out, in_, op, r, w):
        self.S.op("vector", lambda e: e.tensor_reduce(out=out, in_=in_, axis=AX.X, op=op), r, w)

    def recip(self, out, in_, r, w):
        self.S.op("vector", lambda e: e.reciprocal(out=out, in_=in_), r, w)

    def memset(self, eng, ap, v, r, w):
        self.S.op(eng, lambda e: e.memset(ap, v), r, w)


def host_consts():
    pos = np.arange(S_LEN, dtype=np.float32)
    inv = (np.float32(10000.0) ** (-np.arange(0, 128, 2, dtype=np.float32) / np.float32(128))).astype(np.float32)
    ang = (pos[None, :] * inv[:, None]).astype(np.float32)
    cosT = np.concatenate([np.cos(ang), np.cos(ang)], 0).astype(np.float32)
    sinT = np.concatenate([-np.sin(ang), np.sin(ang)], 0).astype(np.float32)
    import ml_dtypes
    pswap = np.zeros((128, 128), np.float32)
    for m in range(128):
        pswap[(m + 64) % 128, m] = 1.0
    ident = np.eye(128, dtype=np.float32)
    j = np.arange(128)[:, None]
    i = np.arange(128)[None, :]
    U = (j <= i).astype(np.float32)
    L = (j > i).astype(np.float32)
    ii = np.arange(128)[:, None]
    jj = np.arange(128)[None, :]
    mprev = np.where(jj >= ii, 0.0, NEG).astype(np.float32)
    mcur = np.where(jj <= ii, 0.0, NEG).astype(np.float32)
    maskA = np.concatenate([mprev, mcur], 1).astype(np.float32)
    bf = ml_dtypes.bfloat16
    return {
        "k_cosT": cosT, "k_sinT": sinT, "k_pswap": pswap.astype(bf), "k_identb": ident.astype(bf),
        "k_identf": ident, "k_Uf": U, "k_Ub": U.astype(bf), "k_Lf": L, "k_maskA": maskA,
    }


def build(debug=(), upto=99):
    b = B(debug, upto)
    nc, S = b.nc, b.S
    x_in = b.dram_in("x", [S_LEN, D])
    c_in = b.dram_in("c", [D])
    w_ada = b.dram_in("w_ada", [DEPTH, D, 6 * D])
    b_ada = b.dram_in("b_ada", [DEPTH, 6 * D])
    norm_mix = b.dram_in("norm_mix", [DEPTH, D])
    norm_ffn = b.dram_in("norm_ffn", [DEPTH, D])
    w_in = b.dram_in("w_in", [DEPTH, D, IN_W])
    w_alpha2 = b.dram_in("w_alpha2", [DEPTH, 16, 512])
    b_alpha2 = b.dram_in("b_alpha2", [DEPTH, 512])
    gla_gain = b.dram_in("gla_gain", [DEPTH, 1024])
    w_out_a = b.dram_in("w_out_a", [DEPTH, 512, D])
    w_out_b = b.dram_in("w_out_b", [DEPTH, 1024, D])
    w_out = b.dram_in("w_out", [DEPTH, D, D])
    w_router = b.dram_in("w_router", [D, 16])
    b_router = b.dram_in("b_router", [16])
    if upto >= 7:
        w_gate_e = b.dram_in("w_gate_e", [DEPTH, 16, D, 1024])
        w_up_e = b.dram_in("w_up_e", [DEPTH, 16, D, 1024])
        w_down_e = b.dram_in("w_down_e", [DEPTH, 16, 1024, D])
    final_norm = b.dram_in("final_norm", [D])
    k_cosT = b.dram_in("k_cosT", [128, S_LEN])
    k_sinT = b.dram_in("k_sinT", [128, S_LEN])
    k_pswap = b.dram_in("k_pswap", [128, 128], BF16)
    k_identb = b.dram_in("k_identb", [128, 128], BF16)
    k_identf = b.dram_in("k_identf", [128, 128])
    k_Uf = b.dram_in("k_Uf", [128, 128])
    k_Ub = b.dram_in("k_Ub", [128, 128], BF16)
    k_Lf = b.dram_in("k_Lf", [128, 128])
    k_maskA = b.dram_in("k_maskA", [128, 256])
    out = b.dram_out("out", [S_LEN, D])

    ada = b.scratch("ada", [DEPTH, 6 * D])
    xs = [x_in] + [b.scratch("xs%d" % i, [S_LEN, D]) for i in range(1, 2 * DEPTH + 1)]
    hTs = b.scratch("hTs", [128, NC_, S_LEN], BF16)
    qaT = b.scratch("qaT", [12, 128, S_LEN], BF16)
    kaT = b.scratch("kaT", [12, 128, S_LEN], BF16)
    va = b.scratch("va", [S_LEN, 1536], BF16)
    qbT = b.scratch("qbT", [4, 128, S_LEN])
    kbT = b.scratch("kbT", [4, 128, S_LEN])
    kbt = b.scratch("kbt", [S_LEN, 512])
    vb = b.scratch("vb", [S_LEN, 1024], BF16)
    rb = b.scratch("rb", [S_LEN, 1024])
    alT = b.scratch("alT", [16, S_LEN])
    sga = b.scratch("sga", [NC_, 128, S_LEN], BF16)
    sgb = b.scratch("sgb", [NC_, 128, S_LEN], BF16)
    oa = [b.scratch("oa%d" % g, [S_LEN, 4, 130]) for g in range(3)]
    yaT_d = b.scratch("yaT", [128, 4, S_LEN], BF16)
    ybT_d = b.scratch("ybT", [128, 8, S_LEN], BF16)
    gates_d = b.scratch("gates", [128, NT, 16])

    identb = b.sb("identb", [128, 128], BF16)
    identf = b.sb("identf", [128, 128], F32)
    persist_end = None
    b.dma("sync", identb, k_identb, [], ["identb"])
    b.dma("sync", identf, k_identf, [], ["identf"])
    gates_p = b.sb("gates", [128, NT, 16], F32)
    persist_end = b.sb_off

    st = contextlib.ExitStack()
    pst = [st.enter_context(nc.psum_tensor("ps%d" % i, [128, 512], F32)) for i in range(8)]
    ps = [t.ap() if hasattr(t, "ap") else t[:] for t in pst]
    psb = [p.bitcast(BF16) for p in ps]

    def PS(i):
        return ("ps", i)

    c_act = b.sb("c_act", [128, NC_], BF16)
    persist_end = b.sb_off

    def make_ada_emitter(l, banks):
        wblk = [b.sb("wada%d_%d" % (l, i), [128, NC_, 512], BF16) for i in range(2)]
        brow = [b.sb("brow%d_%d" % (l, i), [1, 512], F32) for i in range(2)]
        orow = [b.sb("orow%d_%d" % (l, i), [1, 512], F32) for i in range(2)]
        wv = w_ada[l].rearrange("(c p) n -> p c n", p=128)

        def emit(j):
            i = j % 2
            wb = wblk[i]
            wk = ("wada", l, i)
            b.dma("gpsimd", wb, wv[:, :, j * 512:(j + 1) * 512], [], [wk])
            b.dma("sync", brow[i], b_ada[l:l + 1, j * 512:(j + 1) * 512], [], [("brow", l, i)])
            p = ps[banks[i]]
            for k in range(NC_):
                b.mm(p[0:1, :], c_act[:, k:k + 1], wb[:, k, :], k == 0, k == NC_ - 1, [wk, "c_act"], [PS(banks[i])])
            b.tt("vector", orow[i], p[0:1, :], brow[i], ALU.add, [PS(banks[i]), ("brow", l, i)], [("orow", l, i)])
            b.dma("sync", ada[l:l + 1, j * 512:(j + 1) * 512], orow[i], [("orow", l, i)], [])
        return emit

    def phase_ada():
        b.sb_off = persist_end
        c_col = b.sb("c_col", [128, NC_], F32)
        b.dma("sync", c_col, c_in.rearrange("(c p) -> p c", p=128), [], ["c_col"], slow=True)
        b.act(c_act, c_col, AF.Silu, ["c_col"], ["c_act"])
        em = make_ada_emitter(0, (0, 1))
        for j in range(24):
            em(j)
        S.barrier()

    def phase_norm(l, x_src, gain_vec, col_shift, col_scale, hT, router=None, hts=None):
        G = b.sb("G_bc", [128, D], F32)
        A = b.sb("A_bc", [128, D], F32)
        Bc = b.sb("B_bc", [128, D], F32)
        NBX = 4
        xt = [b.sb("xt%d" % i, [128, D], F32) for i in range(NBX)]
        hb = [b.sb("hb%d" % i, [128, D], BF16) for i in range(2)]
        junk = b.sb("junk", [128, D], BF16)
        st_ = b.sb("nstat", [128, NT, 4], F32)
        b.dma("sync", G, gain_vec.partition_broadcast(128), [], ["G"])
        b.dma("sync", A, ada[l, col_scale * D:(col_scale + 1) * D].partition_broadcast(128), [], ["A"])
        b.dma("sync", Bc, ada[l, col_shift * D:(col_shift + 1) * D].partition_broadcast(128), [], ["Bc"])
        b.stt(A, A, 1.0, G, ALU.add, ALU.mult, ["A", "G"], ["A"])
        if router is not None:
            wr, aff_all, hTf = router

        def n1(t):
            i = t % NBX
            xk = ("xt", i)
            sk = ("nst", t)
            b.dma("sync", xt[i], x_src[t * 128:(t + 1) * 128, :], [], [xk])
            b.act(junk, xt[i], AF.Square, [xk], ["junk", sk], accum=st_[:, t, 0:1])
            b.ts("vector", st_[:, t, 1:2], st_[:, t, 0:1], 1.0 / D, EPS, ALU.mult, ALU.add, [sk], [sk])
            b.act(st_[:, t, 2:3], st_[:, t, 1:2], AF.Sqrt, [sk], [sk])
            b.recip(st_[:, t, 3:4], st_[:, t, 2:3], [sk], [sk])

        def n2(t):
            i = t % NBX
            xk = ("xt", i)
            sk = ("nst", t)
            b.stt(xt[i], xt[i], st_[:, t, 3:4], A, ALU.mult, ALU.mult, [xk, sk, "A"], [xk])
            b.tt("gpsimd", xt[i], xt[i], Bc, ALU.add, [xk, "Bc"], [xk])

        def n3(t):
            i = t % NBX
            xk, hk = ("xt", i), ("hb", t % 2)
            hbt = hb[t % 2]
            b.act(hbt, xt[i], AF.Copy, [xk], [hk])
            for q4 in range(4):
                pi = 4 + (t * 4 + q4) % 4
                for cc in range(4):
                    c = q4 * 4 + cc
                    b.tr(psb[pi][:, cc * 128:(cc + 1) * 128], hbt[:, c * 128:(c + 1) * 128], identb, [hk, "identb"], [PS(pi)])
                b.cp_rr(hT[:, q4 * 4:(q4 + 1) * 4, t * 128:(t + 1) * 128],
                        psb[pi][:, 0:512].rearrange("p (c n) -> p c n", c=4), [PS(pi)], [("hT", t)])

        def n4(t):
            i = t % NBX
            xk = ("xt", i)
            for q4 in range(4):
                pi = (t * 4 + q4) % 2
                for cc in range(4):
                    c = q4 * 4 + cc
                    b.tr(ps[pi][:, cc * 128:(cc + 1) * 128], xt[i][:, c * 128:(c + 1) * 128], identf, [xk, "identf"], [PS(pi)])
                b.cp_rr(hTf[:, q4 * 4:(q4 + 1) * 4, :], ps[pi][:, 0:512].rearrange("p (c n) -> p c n", c=4), [PS(pi)], [("hTf", q4)])
            for c in range(NC_):
                b.mm(ps[2][:, 0:16], hTf[:, c, :], wr[:, c, :], c == 0, c == NC_ - 1, [("hTf", c // 4), "wr"], [PS(2)])
            b.act(aff_all[:, t, :], ps[2][:, 0:16], AF.Sigmoid, [PS(2)], ["aff"])

        stages = [n1, n2, n3] + ([n4] if router is not None else [])
        for step in range(NT + len(stages) - 1):
            for si, stg in enumerate(stages):
                idx = step - si
                if 0 <= idx < NT:
                    stg(idx)
        if hts is not None:
            for c in range(NC_):
                b.dma("sync", hts[:, c, :], hT[:, c, :], [("hT", t) for t in range(NT)], [])

    import os
    SEL = os.environ.get("KSEL", "")

    def phase_inproj(l, hT):
        cosT = b.sb("cosT", [128, S_LEN], F32)
        sinT = b.sb("sinT", [128, S_LEN], F32)
        pswap = b.sb("pswap", [128, 128], BF16)
        b.dma("sync", cosT, k_cosT, [], ["cosT"])
        b.dma("sync", sinT, k_sinT, [], ["sinT"])
        b.dma("sync", pswap, k_pswap, [], ["pswap"])
        wblk = [b.sb("wblk%d" % i, [128, NC_, 512], BF16) for i in range(3)]
        ev = [b.sb("ev%d" % i, [128, 512], F32) for i in range(4)]
        evb = [b.sb("evb%d" % i, [128, 512], BF16) for i in range(4)]
        t1 = [b.sb("t1_%d" % i, [128, 512], F32) for i in range(2)]
        t2 = [b.sb("t2_%d" % i, [128, 512], F32) for i in range(2)]
        qsb = [b.sb("qsb%d" % i, [128, 512], BF16) for i in range(2)]
        wv = w_in[l].rearrange("(c p) n -> p c n", p=128)
        hTr = [("hT", t) for t in range(NT)]
        state = {"n": 0, "e": 0, "p": 0, "a": 0}
        ada_em = make_ada_emitter(l + 1, (6, 7)) if l + 1 < DEPTH else None

        def ada_hook():
            if ada_em is not None and state["a"] < 24:
                ada_em(state["a"])
                state["a"] += 1

        def load(col0, ncols=512):
            i = state["n"] % 3
            state["n"] += 1
            b.dma("gpsimd", wblk[i][:, :, 0:ncols], wv[:, :, col0:col0 + ncols], [], [("wblk", i)])
            return wblk[i], ("wblk", i)

        def nextps():
            state["p"] += 1
            return state["p"] % 4

        def fm_block(col0, nchunks, evac, width=128):
            wb, wk = load(col0, 512 if nchunks * width > 16 else 16)
            pend = None
            for cc in range(nchunks):
                for tb in range(4):
                    pi = nextps()
                    for k in range(NC_):
                        b.mm(ps[pi][0:width, :], wb[:, k, cc * width:(cc + 1) * width], hT[:, k, tb * 512:(tb + 1) * 512],
                             k == 0, k == NC_ - 1, [wk] + hTr[tb * 4:(tb + 1) * 4], [PS(pi)])
                    if pend is not None:
                        evac(*pend)
                    pend = (pi, cc, tb)
            evac(*pend)
            ada_hook()

        def tm_block(col0, evac):
            wb, wk = load(col0)
            pend = None
            for t in range(NT):
                pi = nextps()
                for k in range(NC_):
                    b.mm(ps[pi], hT[:, k, t * 128:(t + 1) * 128], wb[:, k, :], k == 0, k == NC_ - 1, [wk, hTr[t]], [PS(pi)])
                if pend is not None:
                    evac(*pend)
                pend = (pi, t)
            evac(*pend)
            ada_hook()

        def nextev():
            state["e"] += 1
            return state["e"] % 4

        def rope_evac(dst, head0):
            def f(pi, cc, tb):
                j = state["e"] % 2
                state["e"] += 1
                tok = slice(tb * 512, (tb + 1) * 512)
                b.act(qsb[j], ps[pi], AF.Copy, [PS(pi)], [("qsb", j)])
                p2 = 4 + j
                VAR = os.environ.get("KVAR", "")
                if VAR != "noswap":
                    b.mm(ps[p2], pswap, qsb[j], True, True, [("qsb", j), "pswap"], [PS(p2)])
                else:
                    p2 = pi
                b.tt("vector", t1[j], ps[pi], cosT[:, tok], ALU.mult, [PS(pi), "cosT", ("qsb", j)], [("t1", j)])
                b.tt("vector", t2[j], ps[p2], sinT[:, tok], ALU.mult, [PS(p2), "sinT"], [("t2", j)])
                b.tt("vector", evb[j], t1[j], t2[j], ALU.add, [("t1", j), ("t2", j)], [("evb", j)])
                b.dma("sync", dst[head0 + cc, :, tok], evb[j], [("evb", j)], [])
            return f

        def plain_fm(dst_fn, func, dt, width=128):
            def f(pi, cc, tb):
                j = nextev()
                tok = slice(tb * 512, (tb + 1) * 512)
                buf = ev[j] if dt == F32 else evb[j]
                key = ("ev", j) if dt == F32 else ("evb", j)
                if func is None:
                    b.cp_rr(buf[0:width, :], ps[pi][0:width, :], [PS(pi)], [key])
                else:
                    b.act(buf[0:width, :], ps[pi][0:width, :], func, [PS(pi)], [key])
                b.dma("sync", dst_fn(cc)[:, tok], buf[0:width, :], [key], [])
            return f

        def plain_tm(dst, col0, func, dt):
            def f(pi, t):
                j = nextev()
                buf = ev[j] if dt == F32 else evb[j]
                key = ("ev", j) if dt == F32 else ("evb", j)
                if func is None:
                    b.cp_rr(buf, ps[pi], [PS(pi)], [key])
                else:
                    b.act(buf, ps[pi], func, [PS(pi)], [key])
                b.dma("sync", dst[t * 128:(t + 1) * 128, col0:col0 + 512], buf, [key], [])
            return f

        def on(c):
            return (not SEL) or (c in SEL)
        if on("a"):
            for g in range(3):
                fm_block(O_QA + g * 512, 4, rope_evac(qaT, g * 4))
            for g in range(3):
                fm_block(O_KA + g * 512, 4, rope_evac(kaT, g * 4))
        if on("b"):
            for g in range(3):
                tm_block(O_VA + g * 512, plain_tm(va, g * 512, None, BF16))
        if on("c"):
            fm_block(O_QB, 4, plain_fm(lambda cc: qbT[cc], None, F32))
            fm_block(O_KB, 4, plain_fm(lambda cc: kbT[cc], None, F32))
            tm_block(O_KB, plain_tm(kbt, 0, None, F32))
        if on("d"):
            for j in range(2):
                tm_block(O_VB + j * 512, plain_tm(vb, j * 512, None, BF16))
            for j in range(2):
                tm_block(O_RB + j * 512, plain_tm(rb, j * 512, AF.Silu, F32))
        if on("e"):
            fm_block(O_AL, 1, plain_fm(lambda cc: alT, None, F32, width=16), width=16)
        if on("f"):
            for j in range(4):
                fm_block(O_GA + j * 512, 4, plain_fm(lambda cc, j=j: sga[j * 4 + cc], AF.Sigmoid, BF16))
            for j in range(4):
                fm_block(O_GB + j * 512, 4, plain_fm(lambda cc, j=j: sgb[j * 4 + cc], AF.Sigmoid, BF16))
        while ada_em is not None and state["a"] < 24:
            ada_hook()

    def phase_attn(l):
        maskA = b.sb("maskA", [128, 256], F32)
        b.dma("sync", maskA, k_maskA, [], ["maskA"])
        qT = [b.sb("qT%d" % i, [128, S_LEN], BF16) for i in range(2)]
        kT = [b.sb("kT%d" % i, [128, S_LEN], BF16) for i in range(2)]
        vp = [b.sb("vp%d" % i, [128, 16, 128], BF16) for i in range(2)]
        ob = [b.sb("ob%d" % i, [128, 16, 130], F32) for i in range(2)]
        NBA = 8
        sm = [b.sb("sm%d" % i, [128, 256], F32) for i in range(NBA)]
        pp = [b.sb("pp%d" % i, [128, 256], BF16) for i in range(NBA)]
        pT = [b.sb("pT%d" % i, [128, 256], BF16) for i in range(NBA)]
        negm = [b.sb("negm%d" % i, [128, 1], F32) for i in range(NBA)]
        scale = 128.0 ** -0.5
        def pipeline(stages, items):
            for step in range(len(items) + len(stages) - 1):
                for si, stg in enumerate(stages):
                    idx = step - si
                    if 0 <= idx < len(items):
                        stg(items[idx])

        def head_load(head):
            g = head // 4
            d = GROUPS[g][1]
            nb = 16 // d
            hi = head % 2
            b.dma("sync", qT[hi], qaT[head], [], [("qT", hi)])
            b.dma("sync", kT[hi], kaT[head], [], [("kT", hi)])
            vsrc = va[:, head * 128:(head + 1) * 128].rearrange("(n j r) c -> j r n c", n=nb, j=128, r=d)
            for r in range(d):
                b.dma("sync", vp[hi][:, r * nb:(r + 1) * nb, :], vsrc[:, r, :, :], [], [("vp", hi)])

        items = []
        for head in range(12):
            g = head // 4
            d = GROUPS[g][1]
            nb = 16 // d
            for r in range(d):
                for n in range(nb):
                    it = dict(head=head, g=g, hh=head % 4, d=d, nb=nb, r=r, n=n, bi=r * nb + n, hi=head % 2,
                              j=len(items) % NBA, seq=len(items) % 16)
                    it["nk"] = 256 if n > 0 else 128
                    it["w0"] = 0 if n > 0 else 128
                    items.append(it)

        def keys(it):
            j, hi = it["j"], it["hi"]
            return ("sm", j), ("pp", j), ("pT", j), ("negm", j), ("ob", hi, it["bi"]), ("qT", hi), ("kT", hi), ("vp", hi)

        def stA(it):
            d, n, r, hi, j, nk = it["d"], it["n"], it["r"], it["hi"], it["j"], it["nk"]
            if it["seq"] == 8 and it["head"] + 1 < 12:
                head_load(it["head"] + 1)
            smk, ppk, pTk, nmk, ok, qk, kk, vk = keys(it)

            def cols(ap_, s0, n_):
                return ap_[:, s0:s0 + (n_ - 1) * d + 1:d] if d > 1 else ap_[:, s0:s0 + n_]
            q0 = d * 128 * n + r
            qcols = cols(qT[hi], q0, 128)
            if n > 0:
                kcols = cols(kT[hi], d * 128 * (n - 1) + r, 256)
            else:
                kcols = cols(kT[hi], q0, 128)
            b.mm(ps[j][:, 0:nk], qcols, kcols, True, True, [qk, kk], [PS(j)])

        def stB(it):
            hi, j, nk, w0, bi = it["hi"], it["j"], it["nk"], it["w0"], it["bi"]
            smk, ppk, pTk, nmk, ok, qk, kk, vk = keys(it)
            b.stt(sm[j][:, 0:nk], ps[j][:, 0:nk], scale, maskA[:, w0:w0 + nk], ALU.mult, ALU.add, [PS(j), "maskA"], [smk])
            b.red(ob[hi][:, bi, 128:129], sm[j][:, 0:nk], ALU.max, [smk], [ok])
            b.ts("vector", negm[j], ob[hi][:, bi, 128:129], -1.0, None, ALU.mult, None, [ok], [nmk])

        def stC(it):
            hi, j, nk, bi = it["hi"], it["j"], it["nk"], it["bi"]
            smk, ppk, pTk, nmk, ok, qk, kk, vk = keys(it)
            b.act(pp[j][:, 0:nk], sm[j][:, 0:nk], AF.Exp, [smk, nmk], [ppk, ok], bias=negm[j], accum=ob[hi][:, bi, 129:130])

        def stD(it):
            j, nk = it["j"], it["nk"]
            smk, ppk, pTk, nmk, ok, qk, kk, vk = keys(it)
            for hlf in range(nk // 128):
                b.tr(psb[j][:, 768 + hlf * 128:768 + (hlf + 1) * 128], pp[j][:, hlf * 128:(hlf + 1) * 128], identb, [ppk, "identb"], [PS(j)])

        def stE(it):
            j, nk = it["j"], it["nk"]
            smk, ppk, pTk, nmk, ok, qk, kk, vk = keys(it)
            b.cp_rr(pT[j][:, 0:nk], psb[j][:, 768:768 + nk], [PS(j)], [pTk])

        def stF(it):
            hi, j, n, bi = it["hi"], it["j"], it["n"], it["bi"]
            smk, ppk, pTk, nmk, ok, qk, kk, vk = keys(it)
            if n > 0:
                b.mm(ps[j][:, 256:384], pT[j][:, 0:128], vp[hi][:, bi - 1, :], True, False, [pTk, vk], [PS(j)])
                b.mm(ps[j][:, 256:384], pT[j][:, 128:256], vp[hi][:, bi, :], False, True, [pTk, vk], [PS(j)])
            else:
                b.mm(ps[j][:, 256:384], pT[j][:, 0:128], vp[hi][:, bi, :], True, True, [pTk, vk], [PS(j)])

        def stG(it):
            hi, j, bi = it["hi"], it["j"], it["bi"]
            smk, ppk, pTk, nmk, ok, qk, kk, vk = keys(it)
            b.cp_rr(ob[hi][:, bi, 0:128], ps[j][:, 256:384], [PS(j)], [ok])
            if it["seq"] == 15:
                g, hh, d, nb = it["g"], it["hh"], it["d"], it["nb"]
                dst = oa[g][:, hh, :].rearrange("(n j r) c -> j r n c", n=nb, j=128, r=d)
                for r in range(d):
                    b.dma("sync", dst[:, r, :, :], ob[hi][:, r * nb:(r + 1) * nb, :], [("ob", hi, x_) for x_ in range(16)], [])

        head_load(0)
        pipeline([stA, stB, stC, stD, stE, stF, stG], items)
        S.barrier()
        og = [[b.sb("og%d_%d" % (i, g), [128, 4, 130], F32) for g in range(3)] for i in range(2)]
        M = [b.sb("M%d" % i, [128, 4], F32) for i in range(2)]
        eg = [b.sb("eg%d" % i, [128, 3, 4], F32) for i in range(2)]
        wd = [b.sb("wd%d" % i, [128, 4], F32) for i in range(2)]
        tmp = [b.sb("tmp%d" % i, [128, 4], F32) for i in range(2)]
        ya = [b.sb("ya%d" % i, [128, 4, 128], F32) for i in range(2)]
        yt = [b.sb("yt%d" % i, [128, 4, 128], F32) for i in range(2)]
        yab = [b.sb("yab%d" % i, [128, 512], BF16) for i in range(2)]
        yaT = b.sb("yaT", [128, 4, S_LEN], BF16)
        for t in range(NT):
            i = t % 2
            ogk = [("og", i, g) for g in range(3)]
            mk = ("mrg", i)
            for g in range(3):
                b.dma("sync", og[i][g], oa[g][t * 128:(t + 1) * 128], [], [ogk[g]])
            m = [og[i][g][:, :, 128] for g in range(3)]
            dn = [og[i][g][:, :, 129] for g in range(3)]
            b.tt("vector", M[i], m[0], m[1], ALU.max, [ogk[0], ogk[1]], [mk])
            b.tt("vector", M[i], M[i], m[2], ALU.max, [mk, ogk[2]], [mk])
            for g in range(3):
                b.tt("vector", eg[i][:, g, :], m[g], M[i], ALU.subtract, [ogk[g], mk], [mk])
            egf = eg[i].rearrange("p g h -> p (g h)")
            b.act(egf, egf, AF.Exp, [mk], [mk])
            b.tt("vector", wd[i], eg[i][:, 0, :], dn[0], ALU.mult, [mk, ogk[0]], [mk])
            for g in (1, 2):
                b.tt("vector", tmp[i], eg[i][:, g, :], dn[g], ALU.mult, [mk, ogk[g]], [mk])
                b.tt("vector", wd[i], wd[i], tmp[i], ALU.add, [mk], [mk])
            b.recip(wd[i], wd[i], [mk], [mk])
            for g in range(3):
                b.tt("vector", eg[i][:, g, :], eg[i][:, g, :], wd[i], ALU.mult, [mk], [mk])
            yk = ("ya", i)
            for g in range(3):
                cb = eg[i][:, g, :].unsqueeze(2).to_broadcast([128, 4, 128])
                dsto = ya[i] if g == 0 else yt[i]
                b.tt("vector", dsto, og[i][g][:, :, 0:128], cb, ALU.mult, [ogk[g], mk], [yk if g == 0 else ("yt", i)])
                if g > 0:
                    b.tt("gpsimd", ya[i], ya[i], yt[i], ALU.add, [yk, ("yt", i)], [yk])
            b.act(yab[i], ya[i].rearrange("p h c -> p (h c)"), AF.Copy, [yk], [("yab", i)])
            pi = 6 + i
            for hh in range(4):
                b.tr(psb[pi][:, hh * 128:(hh + 1) * 128], yab[i][:, hh * 128:(hh + 1) * 128], identb, [("yab", i), "identb"], [PS(pi)])
            b.cp_rr(yaT[:, :, t * 128:(t + 1) * 128], psb[pi][:, 0:512].rearrange("p (c n) -> p c n", c=4), [PS(pi)], [("yaT", t)])
        for hh in range(4):
            b.dma("sync", yaT_d[:, hh, :], yaT[:, hh, :], [("yaT", t_) for t_ in range(NT)], [])

    def phase_gla(l):
        Uf = b.sb("Uf", [128, 128], F32)
        Ub = b.sb("Ub", [128, 128], BF16)
        Lf = b.sb("Lf", [128, 128], F32)
        b.dma("sync", Uf, k_Uf, [], ["Uf"])
        b.dma("sync", Ub, k_Ub, [], ["Ub"])
        b.dma("sync", Lf, k_Lf, [], ["Lf"])
        wa2 = b.sb("wa2", [16, 512], F32)
        ba2 = b.sb("ba2", [1, 512], F32)
        ones1 = b.sb("ones1", [1, 128], F32)
        gain = b.sb("gain", [128, 1024], F32)
        alow = b.sb("alow", [16, S_LEN], F32)
        b.dma("sync", wa2, w_alpha2[l], [], ["wa2"])
        b.dma("sync", ba2, b_alpha2[l:l + 1, :], [], ["ba2"])
        b.memset("vector", ones1, 1.0, [], ["ones1"])
        b.dma("sync", gain, gla_gain[l].partition_broadcast(128), [], ["gain"])
        b.dma("sync", alow, alT, [], ["alow"])
        Sst = [b.sb("Sst%d" % h, [128, 256], F32) for h in range(4)]
        Sbf = [b.sb("Sbf%d" % h, [128, 256], BF16) for h in range(4)]
        for h in range(4):
            b.memset("vector", Sst[h], 0.0, [], [("Sst", h)])
            b.memset("gpsimd", Sbf[h], 0.0, [], [("Sbf", h)])
        ybT = b.sb("ybT", [128, 8, S_LEN], BF16)
        NB = 3
        qf = [b.sb("qf%d" % i, [128, 4, 128], F32) for i in range(NB)]
        kf = [b.sb("kf%d" % i, [128, 4, 128], F32) for i in range(NB)]
        kt = [b.sb("kt%d" % i, [128, 512], F32) for i in range(NB)]
        vt = [b.sb("vt%d" % i, [128, 1024], BF16) for i in range(NB)]
        rt = [b.sb("rt%d" % i, [128, 1024], F32) for i in range(NB)]
        ee = [b.sb("ee%d" % i, [128, 512], F32) for i in range(NB)]
        sp = [b.sb("sp%d" % i, [128, 512], F32) for i in range(NB)]
        NBG = 4
        eb = [b.sb("eb%d" % i, [128, 128], F32) for i in range(NBG)]
        ebi = [b.sb("ebi%d" % i, [128, 128], F32) for i in range(NBG)]
        erv = [b.sb("erv%d" % i, [128, 128], F32) for i in range(NBG)]
        qd = [b.sb("qd%d" % i, [128, 128], BF16) for i in range(NBG)]
        ki = [b.sb("ki%d" % i, [128, 128], BF16) for i in range(NBG)]
        ke = [b.sb("ke%d" % i, [128, 128], BF16) for i in range(NBG)]
        am = [b.sb("am%d" % i, [128, 128], BF16) for i in range(NBG)]
        osb = [b.sb("osb%d" % i, [128, 256], F32) for i in range(NBG)]
        oj = [b.sb("oj%d" % i, [128, 256], F32) for i in range(NBG)]
        gs = [b.sb("gs%d" % i, [128, 4], F32) for i in range(NBG)]
        yb = [b.sb("yb%d" % i, [128, 256], BF16) for i in range(NBG)]

        def prologue(t):
            i = t % NB
            tok = slice(t * 128, (t + 1) * 128)
            b.dma("sync", qf[i], qbT[:, :, tok].rearrange("h p n -> p h n"), [], [("qf", i)])
            b.dma("sync", kf[i], kbT[:, :, tok].rearrange("h p n -> p h n"), [], [("kf", i)])
            b.dma("sync", kt[i], kbt[tok, :], [], [("kt", i)])
            b.dma("sync", vt[i], vb[tok, :], [], [("vt", i)])
            b.dma("sync", rt[i], rb[tok, :], [], [("rt", i)])
            pz = 1
            b.mm(ps[pz], alow[:, tok], wa2, True, False, ["alow", "wa2"], [PS(pz)])
            b.mm(ps[pz], ones1, ba2, False, True, ["ones1", "ba2"], [PS(pz)])
            b.act(ee[i], ps[pz], AF.Exp, [PS(pz)], [("ee", i)], scale=-1.0)
            b.act(sp[i], ee[i], AF.Ln, [("ee", i)], [("sp", i)], bias=1.0)

        units = []
        for t in range(NT):
            for h in range(4):
                units.append(dict(t=t, h=h, i=t % NB, j=len(units) % NBG, tok=slice(t * 128, (t + 1) * 128)))

        def g1(u):
            t, h, i, j = u["t"], u["h"], u["i"], u["j"]
            bA = 2 * j
            hc = slice(h * 128, (h + 1) * 128)
            spk = ("sp", i)
            b.mm(ps[bA][:, 0:128], sp[i][:, hc], Uf, True, True, [spk, "Uf"], [PS(bA)])
            b.mm(ps[bA][:, 128:256], Lf, sp[i][:, hc], True, True, [spk, "Lf"], [PS(bA)])
            ek = ("eb", j)
            b.act(eb[j], ps[bA][:, 0:128], AF.Exp, [PS(bA)], [ek], scale=-1.0 / 16)
            b.act(ebi[j], ps[bA][:, 0:128], AF.Exp, [PS(bA)], [("ebi", j)], scale=1.0 / 16)
            b.act(erv[j], ps[bA][:, 128:256], AF.Exp, [PS(bA)], [("erv", j)], scale=-1.0 / 16)
            b.stt(qd[j], qf[i][:, h, :], 128.0 ** -0.5, eb[j], ALU.mult, ALU.mult, [("qf", i), ek], [("qd", j)])
            b.tt("gpsimd", ki[j], kf[i][:, h, :], ebi[j], ALU.mult, [("kf", i), ("ebi", j)], [("ki", j)])
            b.tt("gpsimd", ke[j], kt[i][:, hc], erv[j], ALU.mult, [("kt", i), ("erv", j)], [("ke", j)])
            if h == 0 and t + 1 < NT:
                prologue(t + 1)

        def g2(u):
            j = u["j"]
            bA = 2 * j
            b.mm(ps[bA][:, 256:384], ki[j], qd[j], True, True, [("ki", j), ("qd", j)], [PS(bA)])
            b.tt("vector", am[j], ps[bA][:, 256:384], Ub, ALU.mult, [PS(bA), "Ub"], [("am", j)])

        def g3(u):
            t, h, i, j = u["t"], u["h"], u["i"], u["j"]
            bB = 2 * j + 1
            ek = ("eb", j)
            vh = vt[i][:, h * 256:(h + 1) * 256]
            b.mm(ps[bB][:, 0:256], am[j], vh, True, False, [("am", j), ("vt", i)], [PS(bB)])
            b.mm(ps[bB][:, 0:256], qd[j], Sbf[h], False, True, [("qd", j), ("Sbf", h)], [PS(bB)])
            b.mm(ps[bB][:, 256:512], ke[j], vh, True, True, [("ke", j), ("vt", i)], [PS(bB)])
            b.stt(Sst[h], Sst[h], eb[j][:, 127:128], ps[bB][:, 256:512], ALU.mult, ALU.add, [("Sst", h), ek, PS(bB)], [("Sst", h)])
            b.act(Sbf[h], Sst[h], AF.Copy, [("Sst", h)], [("Sbf", h)])
            b.act(osb[j], ps[bB][:, 0:256], AF.Copy, [PS(bB)], [("osb", j)])

        def g4(u):
            t, h, i, j, tok = u["t"], u["h"], u["i"], u["j"], u["tok"]
            bA = 2 * j
            ok_ = ("osb", j)
            gk = ("gs", j)
            b.stt(oj[j], osb[j], 1.0, osb[j], ALU.mult, ALU.mult, [ok_], [("oj", j), gk], accum=gs[j][:, 0:1])
            b.ts("vector", gs[j][:, 1:2], gs[j][:, 0:1], 1.0 / 256, EPS, ALU.mult, ALU.add, [gk], [gk])
            b.act(gs[j][:, 2:3], gs[j][:, 1:2], AF.Sqrt, [gk], [gk])
            b.recip(gs[j][:, 3:4], gs[j][:, 2:3], [gk], [gk])
            b.stt(oj[j], osb[j], gs[j][:, 3:4], gain[:, h * 256:(h + 1) * 256], ALU.mult, ALU.mult, [ok_, gk, "gain"], [("oj", j)])
            b.tt("gpsimd", yb[j], oj[j], rt[i][:, h * 256:(h + 1) * 256], ALU.mult, [("oj", j), ("rt", i)], [("yb", j)])
            for c2 in range(2):
                b.tr(psb[bA][:, 768 + c2 * 128:768 + (c2 + 1) * 128], yb[j][:, c2 * 128:(c2 + 1) * 128], identb, [("yb", j), "identb"], [PS(bA)])
            b.cp_rr(ybT[:, h * 2:(h + 1) * 2, tok], psb[bA][:, 768:1024].rearrange("p (c n) -> p c n", c=2), [PS(bA)], [("ybT", t, h)])

        prologue(0)
        for step in range(len(units) + 3):
            for si, stg in enumerate((g1, g2, g3, g4)):
                idx = step - si
                if 0 <= idx < len(units):
                    stg(units[idx])
        for c in range(8):
            b.dma("sync", ybT_d[:, c, :], ybT[:, c, :], [("ybT", t_, c // 2) for t_ in range(NT)], [])

    def phase_out(l, x_src, x_dst):
        mT = b.sb("mT", [128, NC_, S_LEN], BF16)
        mark_o = b.sb_off
        yaT = b.sb("yaT", [128, 4, S_LEN], BF16)
        ybT = b.sb("ybT", [128, 8, S_LEN], BF16)
        for hh in range(4):
            b.dma("sync", yaT[:, hh, :], yaT_d[:, hh, :], [], ["yaT"])
        for c in range(8):
            b.dma("sync", ybT[:, c, :], ybT_d[:, c, :], [], ["ybT"])
        woa = b.sb("woa", [128, 4, D], BF16)
        wob = b.sb("wob", [128, 8, D], BF16)
        b.dma("gpsimd", woa, w_out_a[l].rearrange("(c p) n -> p c n", p=128), [], ["woa"])
        for c in range(8):
            b.dma("gpsimd", wob[:, c, :], w_out_b[l, c * 128:(c + 1) * 128, :], [], ["wob"])
        ga = [b.sb("ga%d" % i, [128, 512], BF16) for i in range(2)]
        gb = [b.sb("gb%d" % i, [128, 512], BF16) for i in range(2)]
        m1 = [b.sb("m1_%d" % i, [128, 512], F32) for i in range(2)]
        m2 = [b.sb("m2_%d" % i, [128, 512], F32) for i in range(2)]
        n = 0
        for dc in range(NC_):
            for tb in range(4):
                j = n % 2
                n += 1
                tok = slice(tb * 512, (tb + 1) * 512)
                b.dma("sync", ga[j], sga[dc, :, tok], [], [("ga", j)])
                b.dma("sync", gb[j], sgb[dc, :, tok], [], [("gb", j)])
                pa, pb = j, 2 + j
                for k in range(4):
                    b.mm(ps[pa], woa[:, k, dc * 128:(dc + 1) * 128], yaT[:, k, tok], k == 0, k == 3, ["woa", "yaT"], [PS(pa)])
                for k in range(8):
                    b.mm(ps[pb], wob[:, k, dc * 128:(dc + 1) * 128], ybT[:, k, tok], k == 0, k == 7, ["wob", "ybT"], [PS(pb)])
                b.tt("vector", m1[j], ps[pa], ga[j], ALU.mult, [PS(pa), ("ga", j)], [("m1", j)])
                b.tt("vector", m2[j], ps[pb], gb[j], ALU.mult, [PS(pb), ("gb", j)], [("m2", j)])
                b.tt("gpsimd", mT[:, dc, tok], m1[j], m2[j], ALU.add, [("m1", j), ("m2", j)], [("mT", tb)])
        S.barrier()
        b.sb_off = mark_o
        gate_bc = b.sb("gate_bc", [128, D], F32)
        b.dma("sync", gate_bc, ada[l, 2 * D:3 * D].partition_broadcast(128), [], ["gate_bc"])
        wo = [b.sb("wo%d" % i, [128, NC_, 512], BF16) for i in range(2)]
        xt = [b.sb("xo%d" % i, [128, 512], F32) for i in range(3)]
        xg = [b.sb("xg%d" % i, [128, 512], F32) for i in range(3)]
        wv = w_out[l].rearrange("(c p) n -> p c n", p=128)
        n = 0
        b.dma("gpsimd", wo[0], wv[:, :, 0:512], [], [("wo", 0)])
        for db in range(4):
            wj = db % 2
            dsl = slice(db * 512, (db + 1) * 512)
            if db + 1 < 4:
                b.dma("gpsimd", wo[(db + 1) % 2], wv[:, :, (db + 1) * 512:(db + 2) * 512], [], [("wo", (db + 1) % 2)])
            for t in range(NT):
                j = n % 3
                pi = 4 + n % 4
                n += 1
                b.dma("sync", xt[j], x_src[t * 128:(t + 1) * 128, dsl], [], [("xo", j)])
                for k in range(NC_):
                    b.mm(ps[pi], mT[:, k, t * 128:(t + 1) * 128], wo[wj][:, k, :], k == 0, k == NC_ - 1, [("wo", wj)], [PS(pi)])
                b.tt("vector", xg[j], ps[pi], gate_bc[:, dsl], ALU.mult, [PS(pi), "gate_bc"], [("xg", j)])
                b.tt("gpsimd", xg[j], xg[j], xt[j], ALU.add, [("xg", j), ("xo", j)], [("xg", j)])
                b.dma("sync", x_dst[t * 128:(t + 1) * 128, dsl], xg[j], [("xg", j)], [])

    def phase_route(aff_all, gates):
        brt = b.sb("brt", [128, 16], F32)
        b.dma("sync", brt, b_router.partition_broadcast(128), [], ["brt"])
        sel = b.sb("sel", [128, NT, 16], F32)
        ps_ = b.sb("pairs", [128, NT, 4, 6], F32)
        gsc = b.sb("gsc", [128, NT, 4], F32)
        m1 = b.sb("rm1", [128, NT, 4], F32)
        m2 = b.sb("rm2", [128, NT, 4], F32)
        gmx = b.sb("gmx", [128, NT], F32)
        gmask = b.sb("gmask", [128, NT, 4], F32)
        emask = b.sb("emask", [128, NT, 16], F32)
        den = b.sb("rden", [128, NT], F32)
        k = "route"
        b.tt("vector", sel, aff_all, brt.unsqueeze(1).to_broadcast([128, NT, 16]), ALU.add, ["aff", "brt"], [k])
        s4 = sel.rearrange("p t (g j) -> p t g j", g=4)
        pairs = [(0, 1), (0, 2), (0, 3), (1, 2), (1, 3), (2, 3)]
        for pi, (a_, c_) in enumerate(pairs):
            b.tt("vector", ps_[:, :, :, pi], s4[:, :, :, a_], s4[:, :, :, c_], ALU.add, [k], [k])
        b.S.op("vector", lambda e: e.tensor_reduce(out=gsc, in_=ps_, axis=AX.X, op=ALU.max), [k], [k])
        b.S.op("vector", lambda e: e.tensor_reduce(out=m1, in_=s4, axis=AX.X, op=ALU.max), [k], [k])
        pm_ = b.sb("pmins", [128, NT, 4, 6], F32)
        for pi, (a_, c_) in enumerate(pairs):
            b.tt("vector", pm_[:, :, :, pi], s4[:, :, :, a_], s4[:, :, :, c_], ALU.min, [k], [k])
        b.S.op("vector", lambda e: e.tensor_reduce(out=m2, in_=pm_, axis=AX.X, op=ALU.max), [k], [k])
        b.S.op("vector", lambda e: e.tensor_reduce(out=gmx, in_=gsc, axis=AX.X, op=ALU.max), [k], [k])
        b.tt("vector", gmask, gsc, gmx.unsqueeze(2).to_broadcast([128, NT, 4]), ALU.is_ge, [k], [k])
        e4 = emask.rearrange("p t (g j) -> p t g j", g=4)
        b.tt("vector", e4, s4, m2.unsqueeze(3).to_broadcast([128, NT, 4, 4]), ALU.is_ge, [k], [k])
        b.tt("vector", e4, e4, gmask.unsqueeze(3).to_broadcast([128, NT, 4, 4]), ALU.mult, [k], [k])
        b.tt("vector", gates, emask, aff_all, ALU.mult, [k, "aff"], ["gates"])
        b.S.op("vector", lambda e: e.tensor_reduce(out=den, in_=gates, axis=AX.X, op=ALU.add), ["gates"], [k])
        b.recip(den, den, [k], [k])
        b.tt("vector", gates, gates, den.unsqueeze(2).to_broadcast([128, NT, 16]), ALU.mult, ["gates", k], ["gates"])
        b.dma("sync", gates_d, gates, ["gates"], [])

    def phase_moe(l, x_src, x_dst, gates, final=None):
        hTh = b.sb("hTh", [128, NC_, 1024], BF16)
        yacc = b.sb("yacc", [128, 8, D], F32)
        aT = [b.sb("aT%d" % i, [128, 8, 1024], BF16) for i in range(2)]
        wg = [b.sb("wg%d" % i, [128, NC_, 256], BF16) for i in range(2)]
        wu = [b.sb("wu%d" % i, [128, NC_, 256], BF16) for i in range(2)]
        wd = [b.sb("wd%d" % i, [128, 8, 512], BF16) for i in range(2)]
        sg = [b.sb("sg%d" % i, [128, 512], F32) for i in range(2)]
        gate_bc = b.sb("gate2_bc", [128, D], F32)
        b.dma("sync", gate_bc, ada[l, 5 * D:6 * D].partition_broadcast(128), [], ["gate_bc"])
        xq = [b.sb("xq%d" % i, [128, 512], F32) for i in range(2)]
        if final is not None:
            fn_bc = b.sb("fn_bc", [128, D], F32)
            b.dma("sync", fn_bc, final_norm.partition_broadcast(128), [], ["fn_bc"])
            fst = b.sb("fst", [128, 8, 4], F32)
            fjunk = b.sb("fjunk", [128, D], BF16)
        nw = 0
        nd = 0
        ns = 0
        np_ = 0
        def load_hTh(half_):
            for c in range(NC_):
                b.dma("sync", hTh[:, c, :], hTs[:, c, half_ * 1024:(half_ + 1) * 1024], [], ["hTh"])

        load_hTh(0)
        for half in range(2):
            for e in range(16):
                ai = e % 2
                ak = ("aT", ai)
                wgv = w_gate_e[l, e].rearrange("(c p) n -> p c n", p=128)
                wuv = w_up_e[l, e].rearrange("(c p) n -> p c n", p=128)
                for fb in range(4):
                    wi = nw % 2
                    nw += 1
                    b.dma("gpsimd", wg[wi], wgv[:, :, fb * 256:(fb + 1) * 256], [], [("wg", wi)])
                    b.dma("gpsimd", wu[wi], wuv[:, :, fb * 256:(fb + 1) * 256], [], [("wu", wi)])
                    for cc in range(2):
                        fch = fb * 2 + cc
                        for tb in range(2):
                            tok = slice(tb * 512, (tb + 1) * 512)
                            pg = (np_ % 2) * 2
                            pu = pg + 1
                            np_ += 1
                            for k in range(NC_):
                                b.mm(ps[pg], wg[wi][:, k, cc * 128:(cc + 1) * 128], hTh[:, k, tok], k == 0, k == NC_ - 1, [("wg", wi), "hTh"], [PS(pg)])
                            for k in range(NC_):
                                b.mm(ps[pu], wu[wi][:, k, cc * 128:(cc + 1) * 128], hTh[:, k, tok], k == 0, k == NC_ - 1, [("wu", wi), "hTh"], [PS(pu)])
                            si = ns % 2
                            ns += 1
                            b.act(sg[si], ps[pg], AF.Silu, [PS(pg)], [("sg", si)])
                            b.tt("vector", aT[ai][:, fch, tok], ps[pu], sg[si], ALU.mult, [PS(pu), ("sg", si)], [ak])
                wdv = w_down_e[l, e].rearrange("(c p) n -> p c n", p=128)
                for db in range(4):
                    di = nd % 2
                    nd += 1
                    dsl = slice(db * 512, (db + 1) * 512)
                    b.dma("gpsimd", wd[di], wdv[:, :, dsl], [], [("wd", di)])
                    for t8 in range(8):
                        t = half * 8 + t8
                        py = 4 + np_ % 4
                        np_ += 1
                        for f in range(8):
                            b.mm(ps[py], aT[ai][:, f, t8 * 128:(t8 + 1) * 128], wd[di][:, f, :], f == 0, f == 7, [ak, ("wd", di)], [PS(py)])
                        yk = ("yacc", t8, db)
                        if e == 0:
                            b.ts("vector", yacc[:, t8, dsl], ps[py], gates[:, t, e:e + 1], None, ALU.mult, None, [PS(py), "gates"], [yk])
                        else:
                            b.stt(yacc[:, t8, dsl], ps[py], gates[:, t, e:e + 1], yacc[:, t8, dsl], ALU.mult, ALU.add, [PS(py), "gates", yk], [yk])
            if half == 0:
                load_hTh(1)
            for t8 in range(8):
                t = half * 8 + t8
                for db in range(4):
                    dsl = slice(db * 512, (db + 1) * 512)
                    j = (t8 * 4 + db) % 2
                    yk = ("yacc", t8, db)
                    b.dma("sync", xq[j], x_src[t * 128:(t + 1) * 128, dsl], [], [("xq", j)])
                    b.tt("vector", yacc[:, t8, dsl], yacc[:, t8, dsl], gate_bc[:, dsl], ALU.mult, [yk, "gate_bc"], [yk])
                    b.tt("vector", yacc[:, t8, dsl], yacc[:, t8, dsl], xq[j], ALU.add, [yk, ("xq", j)], [yk])
                    if final is None:
                        b.dma("sync", x_dst[t * 128:(t + 1) * 128, dsl], yacc[:, t8, dsl], [yk], [])
                if final is not None:
                    yks = [("yacc", t8, db) for db in range(4)]
                    fk = ("fst", t8)
                    b.act(fjunk, yacc[:, t8, :], AF.Square, yks, ["fjunk", fk], accum=fst[:, t8, 0:1])
                    b.ts("vector", fst[:, t8, 1:2], fst[:, t8, 0:1], 1.0 / D, EPS, ALU.mult, ALU.add, [fk], [fk])
                    b.act(fst[:, t8, 2:3], fst[:, t8, 1:2], AF.Sqrt, [fk], [fk])
                    b.recip(fst[:, t8, 3:4], fst[:, t8, 2:3], [fk], [fk])
                    b.stt(yacc[:, t8, :], yacc[:, t8, :], fst[:, t8, 3:4], fn_bc, ALU.mult, ALU.mult, yks + [fk, "fn_bc"], yks)
                    b.dma("sync", final[t * 128:(t + 1) * 128, :], yacc[:, t8, :], yks, [])

    phase_ada()
    if upto >= 1:
        for l in range(DEPTH):
            xa, xb_, xc = xs[2 * l], xs[2 * l + 1], xs[2 * l + 2]
            b.sb_off = persist_end
            hT = b.sb("hT", [128, NC_, S_LEN], BF16)
            mark = b.sb_off
            phase_norm(l, xa, norm_mix[l], 0, 1, hT, hts=(hTs if "hTs" in b.debug else None))
            S.barrier()
            if upto >= 2:
                b.sb_off = mark
                phase_inproj(l, hT)
                S.barrier()
            if upto >= 3:
                b.sb_off = persist_end
                phase_attn(l)
                S.barrier()
            if upto >= 4:
                b.sb_off = persist_end
                phase_gla(l)
                S.barrier()
            if upto >= 5:
                b.sb_off = persist_end
                phase_out(l, xa, xb_)
                S.barrier()
            if upto >= 6:
                b.sb_off = persist_end
                gates = gates_p
                aff_all = b.sb("aff_all", [128, NT, 16], F32)
                wr = b.sb("wr", [128, NC_, 16], F32)
                hTf = b.sb("hTf", [128, NC_, 128], F32)
                b.dma("sync", wr, w_router.rearrange("(c p) e -> p c e", p=128), [], ["wr"])
                hT = b.sb("hT", [128, NC_, S_LEN], BF16)
                mark2 = b.sb_off
                phase_norm(l, xb_, norm_ffn[l], 3, 4, hT, router=(wr, aff_all, hTf), hts=hTs)
                S.barrier()
                b.sb_off = mark2
                phase_route(aff_all, gates)
                S.barrier()
            if upto >= 7:
                b.sb_off = persist_end
                gates = gates_p
                last = (l == DEPTH - 1)
                phase_moe(l, xb_, xc, gates, final=(out if last else None))
                S.barrier()
            if upto < 7 and l == 0:
                break
    if upto < 7:
        b.sb_off = persist_end
        z = b.sb("z", [128, D], F32)
        b.memset("vector", z, 0.0, [], ["z"])
        for t in range(NT):
            b.dma("sync", out[t * 128:(t + 1) * 128, :], z, ["z"], ["out"])
    S.finish()
    S.emit()
    st.close()
    return nc


_CONSTS = None


def kernel(**inputs):
    global _CONSTS
    if _CONSTS is None:
        _CONSTS = host_consts()
    nc = build()
    shared = {k: np.ascontiguousarray(v) for k, v in inputs.items() if k not in ("x", "c")}
    shared.update(_CONSTS)
    in_maps = []
    for i in range(8):
        m = dict(shared)
        m["x"] = np.ascontiguousarray(inputs["x"][i])
        m["c"] = np.ascontiguousarray(inputs["c"][i])
        in_maps.append(m)
    res = run_bass_kernel_spmd(nc, in_maps, core_ids=list(range(8)))
    return np.stack([r["out"] for r in res.results], axis=0).astype(np.float32)
```
